# Optimizing a Trainium2 kernel written in Bass

```python
import jax, jax.numpy as jnp
from jax import lax
import numpy as np

D_MODEL = 1024
BATCH = 4
SEQ = 4096
DEPTH = 1

CTX_LEN = 256
GRID_W = 64
MIX_WIDTH = D_MODEL
MLSTM_HEADS = 4
MLSTM_WIDTH = MIX_WIDTH // 2
MLSTM_HEAD_DIM = MLSTM_WIDTH // MLSTM_HEADS
RET_HEADS = 4
RET_WIDTH = MIX_WIDTH - MLSTM_WIDTH
RET_HEAD_DIM = RET_WIDTH // RET_HEADS
N_GATE_COLS = 4 * MLSTM_HEADS
SPLIT_SIZES = (MLSTM_WIDTH, MLSTM_WIDTH, MLSTM_WIDTH, MLSTM_WIDTH, N_GATE_COLS,
               RET_WIDTH, RET_WIDTH, RET_WIDTH, RET_WIDTH)
IN_COLS = sum(SPLIT_SIZES)
CHUNK = 128
N_EXPERTS = 16
EC_CAPACITY_FACTOR = 2
EXPERT_FF = 2816
ROPE_BASE = 10000.0
EPS = 1e-6

kernel_name = "hybrid_mlstm_retention_ecmoe_dit"


def rmsnorm(x, w):
    xf = x.astype(jnp.float32)
    y = xf * lax.rsqrt(jnp.mean(xf * xf, axis=-1, keepdims=True) + EPS)
    return (y * w.astype(jnp.float32)).astype(x.dtype)


def modulate(x, w, shift, scale):
    return rmsnorm(x, w) * (1 + scale) + shift


def heads(t, n_heads):
    B, T, _ = t.shape
    return t.reshape(B, T, n_heads, -1).transpose(0, 2, 1, 3)


def head_rmsnorm(h, w):
    y = h * lax.rsqrt(jnp.mean(h * h, axis=-1, keepdims=True) + EPS)
    B, H, T, dh = y.shape
    return y.transpose(0, 2, 1, 3).reshape(B, T, H * dh) * w.astype(jnp.float32)


def to_chunks(t):
    B, H, T = t.shape[:3]
    t = t.reshape((B, H, T // CHUNK, CHUNK) + t.shape[3:])
    return jnp.moveaxis(t, 2, 0)


def from_chunks(h):
    N, B, H, L, d = h.shape
    return jnp.moveaxis(h, 0, 2).reshape(B, H, N * L, d)


def flip_if(t, rev):
    return jnp.flip(t, axis=2) if rev else t


def mlstm_scan(q, k, v, ig, lf, state, with_out):
    tril = jnp.tril(jnp.ones((CHUNK, CHUNK), dtype=bool))

    def step(carry, inp):
        C, n, m = carry
        qc, kc, vc, ic, fc = inp
        b = jnp.cumsum(fc, axis=-1)
        bL = b[..., -1]
        g = bL[..., None] - b + ic
        m_new = jnp.maximum(bL + m, jnp.max(g, axis=-1))
        w = jnp.exp(g - m_new[..., None])
        decay = jnp.exp(bL + m - m_new)
        C_new = decay[..., None, None] * C + jnp.einsum('bhs,bhsv,bhsk->bhvk', w, vc, kc)
        n_new = decay[..., None] * n + jnp.einsum('bhs,bhsk->bhk', w, kc)
        if with_out:
            d_log = jnp.where(tril, b[..., :, None] - b[..., None, :] + ic[..., None, :], -jnp.inf)
            inter = b + m[..., None]
            m_t = jnp.maximum(inter, jnp.max(d_log, axis=-1))
            d_w = jnp.exp(d_log - m_t[..., None])
            i_w = jnp.exp(inter - m_t)
            s = jnp.einsum('bhtk,bhsk->bhts', qc, kc) * d_w
            num = jnp.einsum('bhts,bhsv->bhtv', s, vc) + i_w[..., None] * jnp.einsum('bhvk,bhtk->bhtv', C, qc)
            den = jnp.sum(s, axis=-1) + i_w * jnp.einsum('bhk,bhtk->bht', n, qc)
            h = num / jnp.maximum(jnp.abs(den), jnp.exp(-m_t))[..., None]
        else:
            h = None
        return (C_new, n_new, m_new), h

    xs = (to_chunks(q), to_chunks(k), to_chunks(v), to_chunks(ig), to_chunks(lf))
    carry, hs = lax.scan(step, state, xs)
    return (from_chunks(hs) if with_out else None), carry


def retention_scan(q, k, v, lg, state, with_out):
    pos = jnp.arange(CHUNK, dtype=jnp.float32)
    rel = pos[:, None] - pos[None, :]
    d_mat = jnp.where(rel >= 0, jnp.exp(jnp.maximum(rel, 0.0)[None] * lg[:, None, None]), 0.0)
    inter = jnp.exp((pos + 1.0)[None] * lg[:, None])
    k_w = jnp.exp((CHUNK - 1.0 - pos)[None] * lg[:, None])
    d_chunk = jnp.exp(CHUNK * lg)

    def step(R, inp):
        qc, kc, vc = inp
        R_new = d_chunk[None, :, None, None] * R + jnp.einsum('hs,bhsv,bhsk->bhvk', k_w, vc, kc)
        if with_out:
            s = jnp.einsum('bhtk,bhsk->bhts', qc, kc) * d_mat
            out = jnp.einsum('bhts,bhsv->bhtv', s, vc) + inter[None, :, :, None] * jnp.einsum('bhvk,bhtk->bhtv', R, qc)
        else:
            out = None
        return R_new, out

    R_fin, outs = lax.scan(step, state, (to_chunks(q), to_chunks(k), to_chunks(v)))
    return (from_chunks(outs) if with_out else None), R_fin


def rotate_pairs(t, cos, sin):
    half = t.shape[-1] // 2
    t1, t2 = t[..., :half], t[..., half:]
    return jnp.concatenate([t1 * cos - t2 * sin, t1 * sin + t2 * cos], axis=-1)


def axial_rope(t, rope_cs):
    cr, sr, cc, sc = rope_cs
    half = t.shape[-1] // 2
    return jnp.concatenate([rotate_pairs(t[..., :half], cr, sr),
                            rotate_pairs(t[..., half:], cc, sc)], axis=-1)


def split_proj(p):
    idx = np.cumsum(SPLIT_SIZES)[:-1].tolist()
    return jnp.split(p, idx, axis=-1)


def mlstm_inputs(parts, gate_bias):
    mq, mk, mv, _, mg = parts[:5]
    B, T, _ = mq.shape
    q = heads(mq, MLSTM_HEADS).astype(jnp.float32)
    k = heads(mk, MLSTM_HEADS).astype(jnp.float32) * (MLSTM_HEAD_DIM ** -0.5)
    v = heads(mv, MLSTM_HEADS).astype(jnp.float32)
    g = (mg + gate_bias).astype(jnp.float32).reshape(B, T, 4, MLSTM_HEADS).transpose(2, 0, 3, 1)
    return q, k, v, g[:2], jax.nn.log_sigmoid(g[2:])


def retention_inputs(parts, rope_cs):
    rq, rk, rv = parts[5:8]
    q = heads(rq, RET_HEADS).astype(jnp.float32)
    k = heads(rk, RET_HEADS).astype(jnp.float32) * (RET_HEAD_DIM ** -0.5)
    if rope_cs is not None:
        q, k = axial_rope(q, rope_cs), axial_rope(k, rope_cs)
    return q, k, heads(rv, RET_HEADS).astype(jnp.float32)


def token_mix(hc, hl, w_in, gate_bias, decay_logit, mlstm_norm_w, ret_norm_w, w_out, rope_cs, need_ctx_out):
    B = hl.shape[0]
    pc, pl = split_proj(hc @ w_in), split_proj(hl @ w_in)

    cq, ck, cv, cig, clf = mlstm_inputs(pc, gate_bias)
    lq, lk, lv, lig, llf = mlstm_inputs(pl, gate_bias)
    m_lat, m_ctx = 0.0, 0.0
    for d in range(2):
        rev = d == 1
        st0 = (jnp.zeros((B, MLSTM_HEADS, MLSTM_HEAD_DIM, MLSTM_HEAD_DIM), jnp.float32),
               jnp.zeros((B, MLSTM_HEADS, MLSTM_HEAD_DIM), jnp.float32),
               jnp.zeros((B, MLSTM_HEADS), jnp.float32))
        h_c, st = mlstm_scan(flip_if(cq, rev), flip_if(ck, rev), flip_if(cv, rev),
                             flip_if(cig[d], rev), flip_if(clf[d], rev), st0, need_ctx_out)
        h_l, _ = mlstm_scan(flip_if(lq, rev), flip_if(lk, rev), flip_if(lv, rev),
                            flip_if(lig[d], rev), flip_if(llf[d], rev), st, True)
        m_lat = m_lat + flip_if(h_l, rev)
        if need_ctx_out:
            m_ctx = m_ctx + flip_if(h_c, rev)

    lg = jax.nn.log_sigmoid(decay_logit.astype(jnp.float32))
    rcq, rck, rcv = retention_inputs(pc, None)
    rlq, rlk, rlv = retention_inputs(pl, rope_cs)
    r_lat, r_ctx = 0.0, 0.0
    for d in range(2):
        rev = d == 1
        R0 = jnp.zeros((B, RET_HEADS, RET_HEAD_DIM, RET_HEAD_DIM), jnp.float32)
        o_c, R = retention_scan(flip_if(rcq, rev), flip_if(rck, rev), flip_if(rcv, rev), lg[d], R0, need_ctx_out)
        o_l, _ = retention_scan(flip_if(rlq, rev), flip_if(rlk, rev), flip_if(rlv, rev), lg[d], R, True)
        r_lat = r_lat + flip_if(o_l, rev)
        if need_ctx_out:
            r_ctx = r_ctx + flip_if(o_c, rev)

    def merge(parts, m_sum, r_sum, dtype):
        y_m = jax.nn.sigmoid(parts[3].astype(jnp.float32)) * head_rmsnorm(m_sum, mlstm_norm_w)
        y_r = jax.nn.silu(parts[8].astype(jnp.float32)) * head_rmsnorm(r_sum, ret_norm_w)
        return jnp.concatenate([y_m, y_r], axis=-1).astype(dtype) @ w_out

    y_lat = merge(pl, m_lat, r_lat, hl.dtype)
    y_ctx = merge(pc, m_ctx, r_ctx, hc.dtype) if need_ctx_out else None
    return y_ctx, y_lat


def expert_choice_ffn(h, w_router, w_gate, w_up, w_down):
    B, T, D = h.shape
    cap = EC_CAPACITY_FACTOR * T // N_EXPERTS
    aff = jax.nn.softmax((h @ w_router).astype(jnp.float32), axis=-1)
    gate, idx = lax.top_k(aff.transpose(0, 2, 1), cap)
    xg = jax.vmap(lambda hb, ib: hb[ib])(h, idx)
    a = jnp.einsum('becd,edf->becf', xg, w_gate)
    u = jnp.einsum('becd,edf->becf', xg, w_up)
    y = jnp.einsum('becf,efd->becd', jax.nn.silu(a) * u, w_down) * gate[..., None].astype(h.dtype)
    return jax.vmap(lambda yb, ib: jnp.zeros((T, D), yb.dtype).at[ib.reshape(-1)].add(yb.reshape(-1, D)))(y, idx)


def setup_inputs(seed: int = 0) -> dict:
    key = jax.random.key(seed)
    ks = jax.random.split(key, 22)
    f32 = jnp.float32

    def nrm(k, shape, s):
        return jax.random.normal(k, shape, f32) * s

    x = nrm(ks[0], (BATCH, SEQ, D_MODEL), 1.0)
    c = nrm(ks[1], (BATCH, D_MODEL), 1.0)
    ctx = nrm(ks[2], (BATCH, CTX_LEN, D_MODEL), 1.0)
    c_ctx = nrm(ks[3], (D_MODEL,), 1.0)
    w_ada = nrm(ks[4], (DEPTH, D_MODEL, 6 * D_MODEL), 0.5 * D_MODEL ** -0.5)
    b_ada = nrm(ks[5], (DEPTH, 6 * D_MODEL), 0.02)
    norm1_w = 1.0 + nrm(ks[6], (DEPTH, D_MODEL), 0.02)
    norm2_w = 1.0 + nrm(ks[7], (DEPTH, D_MODEL), 0.02)
    w_in = nrm(ks[8], (DEPTH, D_MODEL, IN_COLS), D_MODEL ** -0.5)
    i_bias = nrm(ks[9], (DEPTH, 2 * MLSTM_HEADS), 0.1)
    f_bias = jnp.tile(jnp.linspace(3.0, 6.0, MLSTM_HEADS, dtype=f32), 2)[None] + nrm(ks[10], (DEPTH, 2 * MLSTM_HEADS), 0.1)
    mlstm_gate_bias = jnp.concatenate([i_bias, f_bias], axis=-1)
    base_logit = jnp.log(2.0 ** jnp.arange(5, 5 + RET_HEADS, dtype=f32) - 1.0)
    ret_decay_logit = base_logit[None, None] + nrm(ks[11], (DEPTH, 2, RET_HEADS), 0.05)
    mlstm_norm_w = 1.0 + nrm(ks[12], (DEPTH, MLSTM_WIDTH), 0.02)
    ret_norm_w = 1.0 + nrm(ks[13], (DEPTH, RET_WIDTH), 0.02)
    w_out = nrm(ks[14], (DEPTH, MIX_WIDTH, D_MODEL), MIX_WIDTH ** -0.5)
    w_router = nrm(ks[15], (DEPTH, D_MODEL, N_EXPERTS), D_MODEL ** -0.5)
    w_gate = nrm(ks[16], (DEPTH, N_EXPERTS, D_MODEL, EXPERT_FF), D_MODEL ** -0.5)
    w_up = nrm(ks[17], (DEPTH, N_EXPERTS, D_MODEL, EXPERT_FF), D_MODEL ** -0.5)
    w_down = nrm(ks[18], (DEPTH, N_EXPERTS, EXPERT_FF, D_MODEL), EXPERT_FF ** -0.5)
    final_norm_w = 1.0 + nrm(ks[19], (D_MODEL,), 0.02)
    return {"x": x, "c": c, "ctx": ctx, "c_ctx": c_ctx, "w_ada": w_ada, "b_ada": b_ada,
            "norm1_w": norm1_w, "norm2_w": norm2_w, "w_in": w_in, "mlstm_gate_bias": mlstm_gate_bias,
            "ret_decay_logit": ret_decay_logit, "mlstm_norm_w": mlstm_norm_w, "ret_norm_w": ret_norm_w,
            "w_out": w_out, "w_router": w_router, "w_gate": w_gate, "w_up": w_up, "w_down": w_down,
            "final_norm_w": final_norm_w}


def reference(x, c, ctx, c_ctx, w_ada, b_ada, norm1_w, norm2_w, w_in, mlstm_gate_bias,
              ret_decay_logit, mlstm_norm_w, ret_norm_w, w_out, w_router, w_gate, w_up, w_down,
              final_norm_w):
    T = x.shape[1]
    ROWS = T // GRID_W
    rows = jnp.repeat(jnp.arange(ROWS, dtype=jnp.float32), GRID_W)
    cols = jnp.tile(jnp.arange(GRID_W, dtype=jnp.float32), ROWS)
    n_freq = RET_HEAD_DIM // 4
    inv_freq = ROPE_BASE ** (-jnp.arange(n_freq, dtype=jnp.float32) / n_freq)
    ang_r, ang_c = rows[:, None] * inv_freq, cols[:, None] * inv_freq
    rope_cs = (jnp.cos(ang_r), jnp.sin(ang_r), jnp.cos(ang_c), jnp.sin(ang_c))

    xl, xc = x, ctx
    for l in range(DEPTH):
        last = l == DEPTH - 1
        mod_l = jax.nn.silu(c) @ w_ada[l] + b_ada[l]
        mod_c = jax.nn.silu(c_ctx) @ w_ada[l] + b_ada[l]
        sh1, sc1, g1, sh2, sc2, g2 = jnp.split(mod_l[:, None, :], 6, axis=-1)
        csh1, csc1, cg1, csh2, csc2, cg2 = jnp.split(mod_c, 6, axis=-1)

        hl = modulate(xl, norm1_w[l], sh1, sc1)
        hc = modulate(xc, norm1_w[l], csh1, csc1)
        y_ctx, y_lat = token_mix(hc, hl, w_in[l], mlstm_gate_bias[l], ret_decay_logit[l],
                                 mlstm_norm_w[l], ret_norm_w[l], w_out[l], rope_cs, not last)
        xl = xl + g1 * y_lat
        xl = xl + g2 * expert_choice_ffn(modulate(xl, norm2_w[l], sh2, sc2),
                                         w_router[l], w_gate[l], w_up[l], w_down[l])
        if not last:
            xc = xc + cg1 * y_ctx
            xc = xc + cg2 * expert_choice_ffn(modulate(xc, norm2_w[l], csh2, csc2),
                                              w_router[l], w_gate[l], w_up[l], w_down[l])
    return rmsnorm(xl, final_norm_w)
```

```python
import numpy as np
import ml_dtypes
from contextlib import ExitStack
import concourse.bass as bass
import concourse.mybir as mybir
from concourse.bass_utils import run_bass_kernel_spmd

F32 = mybir.dt.float32
BF16 = mybir.dt.bfloat16
I32 = mybir.dt.int32
U8 = mybir.dt.uint8
AF = mybir.ActivationFunctionType
ALU = mybir.AluOpType
AX = mybir.AxisListType
DSZ = {F32: 4, BF16: 2, I32: 4, U8: 1}

D = 1024
T = 4096
TC = 256
TA = T + TC
NCH = TA // 128
NL = T // 128
FF = 2816
NFC = FF // 128
NE = 16
CAP = 512
EPS = 1e-6
KS = 128 ** -0.5
ROWW = 1058
CFW = 6 * 128 + 8
NIT = 36
NU = 4
RG = [[0, 1], [2, 3], [4, 5], [6, 7]]


class Arena:
    def __init__(self, ap, size):
        self.ap, self.size, self.top = ap, size, 0

    def alloc(self, shape, dt):
        n = int(np.prod(shape)) * DSZ[dt]
        off = (self.top + 63) // 64 * 64
        assert off + n <= self.size, ("SBUF arena overflow", off + n, self.size)
        self.top = off + n
        v = self.ap[:, off:off + n].bitcast(dt)
        if len(shape) == 2:
            v = v.rearrange("p (a b) -> p a b", a=shape[0])
        elif len(shape) == 3:
            v = v.rearrange("p (a b c) -> p a b c", a=shape[0], b=shape[1])
        return v


class Sched:
    ENG = ['pe', 'dve', 'act', 'pool', 'sp']

    def __init__(self, nc, es, ndma=48):
        self.nc = nc
        self.sem = {e: es.enter_context(nc.semaphore('s_' + e)) for e in self.ENG}
        self.dsem = [es.enter_context(nc.semaphore('d%d' % i)) for i in range(ndma)]
        self.dcnt = [0] * ndma
        self.dnext = 0
        self.csem = es.enter_context(nc.semaphore('s_cc'))
        self.ccnt = 0
        self.cnt = {e: 0 for e in self.ENG}
        self.streams = {e: [] for e in self.ENG}
        self.waited = {e: {} for e in self.ENG}
        self.bufs = {}

    def op(self, eng, fn, reads=(), writes=(), dma=False, extra=(), cc=False):
        deps = set(extra)
        for k in reads:
            b = self.bufs.get(k)
            if b and b[0] is not None:
                deps.add(b[0])
        for k in writes:
            b = self.bufs.get(k)
            if b:
                if b[0] is not None:
                    deps.add(b[0])
                deps.update(b[1])
        if cc:
            self.ccnt += 1
            me = (('c', 0), self.ccnt)
        elif dma:
            slot = self.dnext
            self.dnext = (self.dnext + 1) % len(self.dsem)
            prev = self.dcnt[slot]
            if prev > 0:
                deps.add((('d', slot), prev))
            self.dcnt[slot] = prev + 16
            me = (('d', slot), prev + 16)
        else:
            self.cnt[eng] += 1
            me = (('e', eng), self.cnt[eng])
        best = {}
        for (sk, v) in deps:
            if v > best.get(sk, 0):
                best[sk] = v
        w = self.waited[eng]
        waits = []
        for sk, v in best.items():
            if sk == ('e', 'pe') and eng == 'pe':
                continue
            if w.get(sk, 0) < v:
                w[sk] = v
                waits.append((sk, v))
        self.streams[eng].append((fn, waits, me))
        for k in reads:
            self.bufs.setdefault(k, [None, []])[1].append(me)
        for k in writes:
            self.bufs[k] = [me, []]
        return me

    def barrier(self):
        allv = [(('e', e), self.cnt[e]) for e in self.ENG if self.cnt[e] > 0]
        allv += [(('d', i), v) for i, v in enumerate(self.dcnt) if v > 0]
        if self.ccnt > 0:
            allv.append((('c', 0), self.ccnt))
        for eng in self.ENG:
            w = self.waited[eng]
            waits = []
            for sk, v in allv:
                if sk == ('e', eng):
                    continue
                if w.get(sk, 0) < v:
                    w[sk] = v
                    waits.append((sk, v))
            self.streams[eng].append((None, waits, None))
        self.bufs = {}

    def emit(self):
        nc = self.nc
        S = self

        def run(name, eo):
            for fn, waits, me in S.streams[name]:
                for sk, v in waits:
                    s = S.sem[sk[1]] if sk[0] == 'e' else (S.csem if sk[0] == 'c' else S.dsem[sk[1]])
                    eo.wait_ge(s, v)
                if fn is None:
                    continue
                ins = fn(eo)
                if me[0][0] == 'e':
                    ins.then_inc(S.sem[me[0][1]], 1)
                elif me[0][0] == 'c':
                    ins.then_inc(S.csem)
                else:
                    ins.then_inc(S.dsem[me[0][1]], 16)

        with nc.Block() as block:
            @block.tensor
            def _(e):
                run('pe', e)

            @block.vector
            def _(e):
                run('dve', e)

            @block.scalar
            def _(e):
                run('act', e)

            @block.gpsimd
            def _(e):
                run('pool', e)

            @block.sync
            def _(e):
                run('sp', e)


def build_nc(NEL, use_ar=False, stop_after=None, dbg=None):
    nc = bass.Bass("TRN2", target_bir_lowering=False)

    def din(name, shape, dt=F32):
        return nc.dram_tensor(name, shape, dt, kind="ExternalInput").ap()

    xa = din("xa", [TA, D])
    cvec = din("cvec", [128, 16])
    w_ada = din("w_ada", [D, 6 * D])
    b_ada = din("b_ada", [1, 6 * D])
    nw = din("nw", [3, D])
    winh = din("winh", [NU, D, 768])
    wg = din("wg", [D, 16])
    gbias = din("gbias", [1, 16])
    dlog = din("dlog", [1, 8])
    hnw = din("hnw", [1, NU * 128])
    yflag = din("yflag", [1, 2])
    w_out = din("w_out", [D, D])
    w_router = din("w_router", [D, 16])
    w_gate = din("w_gate", [NEL, D, FF])
    w_up = din("w_up", [NEL, D, FF])
    w_down = din("w_down", [NEL, FF, D])
    rope = din("rope", [2, 128, TA])
    cf = din("cf", [128, CFW])
    ci = din("ci", [128, NL], I32)
    flag = din("flag", [1, 1])
    out = nc.dram_tensor("out", [T, D], F32, kind="ExternalOutput").ap()
    y_full = nc.dram_tensor("y_full", [T, D], F32).ap()
    y_ar = nc.dram_tensor("y_ar", [T, D], F32).ap()
    xl_scr = nc.dram_tensor("xl_scr", [T, D], F32).ap()
    mod_scr = nc.dram_tensor("mod_scr", [128, 6 * D], F32, kind="Internal").ap()
    xg_scr = nc.dram_tensor("xg_scr", [NEL, CAP, ROWW], BF16, kind="Internal").ap()
    xg_flat = xg_scr.rearrange("e c n -> (e c) n")
    xl_ar = nc.dram_tensor("xl_ar", [T, D], F32).ap() if use_ar else xl_scr
    dbg_out = None
    if dbg is not None:
        dbg_out = nc.dram_tensor("dbg", [128, dbg[1]], F32, kind="ExternalOutput").ap()

    es = ExitStack()
    ARENA = 210000
    arena_t = es.enter_context(nc.sbuf_tensor("arena", [128, ARENA], U8))
    ps = es.enter_context(nc.psum_tensor("ps", [128, 4096], F32))
    S = Sched(nc, es)
    A = Arena(arena_t, ARENA)

    def bank(b):
        return ps[:, b * 512:(b + 1) * 512]

    def bankbf(b):
        return ps[:, b * 512:(b + 1) * 512].bitcast(BF16)

    def BK(b):
        return 'bank%d' % b

    def dma(q, out_, in_, reads, writes):
        return S.op(q, lambda e: e.dma_start(out=out_, in_=in_), reads, writes, dma=True)

    def tt(eng, out_, in0, in1, op, reads, writes):
        return S.op(eng, lambda e: e.tensor_tensor(out=out_, in0=in0, in1=in1, op=op), reads, writes)

    def ts(eng, out_, in0, s1, op0, reads, writes, s2=None, op1=None, accum=None):
        if op1 is None:
            return S.op(eng, lambda e: e.tensor_scalar(out=out_, in0=in0, scalar1=s1, scalar2=None, op0=op0),
                        reads, writes)
        return S.op(eng, lambda e: e.tensor_scalar(out=out_, in0=in0, scalar1=s1, scalar2=s2, op0=op0, op1=op1,
                                                   accum_out=accum), reads, writes)

    def stt(out_, in0, scalar, in1, op0, op1, reads, writes):
        return S.op('dve', lambda e: e.scalar_tensor_tensor(out=out_, in0=in0, scalar=scalar, in1=in1,
                                                            op0=op0, op1=op1), reads, writes)

    def act(out_, in_, func, reads, writes, bias=None, scale=None, accum=None):
        def fn(e):
            kw = {}
            if bias is not None:
                kw['bias'] = bias
            if scale is not None:
                kw['scale'] = scale
            if accum is not None:
                kw['accum_out'] = accum
            return e.activation(out=out_, in_=in_, func=func, **kw)
        return S.op('act', fn, reads, writes)

    def cp(eng, out_, in_, reads, writes):
        if eng == 'act':
            return act(out_, in_, AF.Copy, reads, writes)
        return S.op(eng, lambda e: e.tensor_copy(out=out_, in_=in_), reads, writes)

    def recip(out_, in_, reads, writes):
        return S.op('dve', lambda e: e.reciprocal(out=out_, in_=in_), reads, writes)

    def memset(eng, ap, val, writes):
        return S.op(eng, lambda e: e.memset(ap, val), (), writes)

    def mmg(out_, pairs, reads, writes):
        def fn(e):
            ins = None
            n = len(pairs)
            for i, (l, r) in enumerate(pairs):
                ins = e.matmul(out_, l, r, start=(i == 0), stop=(i == n - 1))
            return ins
        return S.op('pe', fn, reads, writes)

    def trg(items, reads, writes):
        def fn(e):
            ins = None
            for (o, i_, idn) in items:
                ins = e.transpose(o, i_, idn)
            return ins
        return S.op('pe', fn, reads, writes)

    cfT = A.alloc([CFW], F32)
    dma('sp', cfT, cf, (), ['cf'])
    ident = cfT[:, 0:128]
    ones = cfT[:, 128:256]
    maskF = cfT[:, 256:384]
    maskB = cfT[:, 384:512]
    SU = cfT[:, 512:640]
    SL = cfT[:, 640:768]
    posw = cfT[:, 768:776]
    identb = A.alloc([128], BF16)
    cp('dve', identb, ident, ['cf'], ['identb'])
    ciT = A.alloc([NL], I32)
    dma('sp', ciT, ci, (), ['ci'])
    flagc = A.alloc([1], F32)
    dma('sp', flagc, flag[0].partition_broadcast(128), (), ['flag'])
    yfl = A.alloc([2], F32)
    dma('sp', yfl, yflag[0].partition_broadcast(128), (), ['yfl'])
    mark0 = A.top
    hT = A.alloc([8, TA], BF16)
    markH = A.top

    cv = A.alloc([16], F32)
    sv = A.alloc([16], F32)
    dma('sp', cv, cvec, (), ['cv'])
    act(sv, cv, AF.Silu, ['cv'], ['sv'])
    lhl = A.alloc([8, 128], F32)
    lhc = A.alloc([8, 128], F32)
    for k in range(8):
        ts('dve', lhl[:, k, :], ones, sv[:, k:k + 1], ALU.mult, ['sv', 'cf'], ['lhl'])
        ts('dve', lhc[:, k, :], ones, sv[:, 8 + k:9 + k], ALU.mult, ['sv', 'cf'], ['lhc'])
    modl = A.alloc([6 * D], F32)
    modc = A.alloc([2 * D], F32)
    wa = [A.alloc([8, 512], F32) for _ in range(2)]
    ba = [A.alloc([512], F32) for _ in range(2)]
    for nb in range(12):
        j = nb % 2
        dma('sp', wa[j], w_ada[:, nb * 512:(nb + 1) * 512].rearrange("(k p) n -> p k n", p=128), (), ['wa%d' % j])
        dma('sp', ba[j][0:1, :], b_ada[0:1, nb * 512:(nb + 1) * 512], (), ['ba%d' % j])
        prs = [(lhl[:, k, :], wa[j][:, k, :]) for k in range(8)] + [(ones[0:1, :], ba[j][0:1, :])]
        mmg(bank(j), prs, ['lhl', 'wa%d' % j, 'ba%d' % j, 'cf'], [BK(j)])
        cp('act', modl[:, nb * 512:(nb + 1) * 512], bank(j), [BK(j)], ['modl'])
        if nb < 4:
            prs = [(lhc[:, k, :], wa[j][:, k, :]) for k in range(8)] + [(ones[0:1, :], ba[j][0:1, :])]
            mmg(bank(2 + j), prs, ['lhc', 'wa%d' % j, 'ba%d' % j, 'cf'], [BK(2 + j)])
            cp('act', modc[:, nb * 512:(nb + 1) * 512], bank(2 + j), [BK(2 + j)], ['modc'])
    nwb = A.alloc([2, D], F32)
    dma('sp', nwb[:, 0, :], nw[0].partition_broadcast(128), (), ['nwb'])
    dma('sp', nwb[:, 1, :], nw[1].partition_broadcast(128), (), ['nwb'])
    stt(modl[:, D:2 * D], modl[:, D:2 * D], 1.0, nwb[:, 0, :], ALU.add, ALU.mult, ['modl', 'nwb'], ['modl'])
    stt(modc[:, D:2 * D], modc[:, D:2 * D], 1.0, nwb[:, 0, :], ALU.add, ALU.mult, ['modc', 'nwb'], ['modc'])
    stt(modl[:, 4 * D:5 * D], modl[:, 4 * D:5 * D], 1.0, nwb[:, 1, :], ALU.add, ALU.mult, ['modl', 'nwb'], ['modl'])
    dma('sp', mod_scr, modl, ['modl'], ['mod_scr'])

    xt = [A.alloc([D], F32) for _ in range(2)]
    t1 = [A.alloc([D], F32) for _ in range(2)]
    hb = [A.alloc([D], BF16) for _ in range(2)]
    junk = A.alloc([D], BF16)
    ssA = A.alloc([NCH], F32)
    sdA = A.alloc([NCH], F32)
    rsA = A.alloc([NCH], F32)
    for i in range(NCH):
        j = i % 2
        W1 = modc[:, D:2 * D] if i < 2 else modl[:, D:2 * D]
        B1 = modc[:, 0:D] if i < 2 else modl[:, 0:D]
        dma('sp', xt[j], xa[i * 128:(i + 1) * 128, :], (), ['xt%d' % j])
        act(junk, xt[j], AF.Square, ['xt%d' % j], ['junk', 'ssA%d' % i], accum=ssA[:, i:i + 1])
        act(sdA[:, i:i + 1], ssA[:, i:i + 1], AF.Sqrt, ['ssA%d' % i], ['sdA%d' % i], bias=EPS, scale=1.0 / D)
        recip(rsA[:, i:i + 1], sdA[:, i:i + 1], ['sdA%d' % i], ['rsA%d' % i])
        stt(t1[j], xt[j], rsA[:, i:i + 1], W1, ALU.mult, ALU.mult, ['xt%d' % j, 'rsA%d' % i, 'modl', 'modc'],
            ['t1%d' % j])
        tt('pool', hb[j], t1[j], B1, ALU.add, ['t1%d' % j, 'modl', 'modc'], ['hb%d' % j])
        b = 4 + j
        trg([(bankbf(b)[:, k * 128:(k + 1) * 128], hb[j][:, k * 128:(k + 1) * 128], identb) for k in range(8)],
            ['hb%d' % j, 'identb'], [BK(b)])
        cp('act', hT[:, :, i * 128:(i + 1) * 128], bankbf(b).rearrange("p (k t) -> p k t", k=8), [BK(b)], ['hT'])
    S.barrier()
    if stop_after == 'B':
        return _finish(nc, S, es, dbg, dbg_out, hT)
    A.top = markH

    wgb = A.alloc([8, 16], BF16)
    S.op('pool', lambda e: e.dma_start(out=wgb, in_=wg.rearrange("(k p) n -> p k n", p=128)), (), ['wgb'], dma=True)
    gb = A.alloc([16], F32)
    dma('sp', gb, gbias[0].partition_broadcast(128), (), ['gb'])
    lgb = A.alloc([8], F32)
    dma('sp', lgb, dlog[0].partition_broadcast(128), (), ['lgb'])
    hnwb = A.alloc([NU * 128], F32)
    dma('sp', hnwb, hnw[0].partition_broadcast(128), (), ['hnwb'])
    Gtok = A.alloc([NCH, 16], F32)
    Lt = A.alloc([NCH, 8], F32)
    tE = A.alloc([NCH, 8], F32)
    WC = A.alloc([NCH, 8], F32)
    LB = A.alloc([NCH, 8], F32)
    PHI = A.alloc([NCH, 8], F32)
    WCR = A.alloc([8], F32)
    RFR = A.alloc([8], F32)
    PHIR = A.alloc([8], F32)
    tR = A.alloc([8], F32)
    tR2 = A.alloc([8], F32)
    for c in range(NCH):
        if c < 32:
            o = bank(0)[:, c * 16:(c + 1) * 16]
            bk = BK(0)
        else:
            o = bank(1)[:, (c - 32) * 16:(c - 31) * 16]
            bk = BK(1)
        mmg(o, [(hT[:, k, c * 128:(c + 1) * 128], wgb[:, k, :]) for k in range(8)], ['hT', 'wgb'], [bk])
    gbb32 = gb[:, 0:16].unsqueeze(1).to_broadcast([128, 32, 16])
    gbb2 = gb[:, 0:16].unsqueeze(1).to_broadcast([128, 2, 16])
    tt('dve', Gtok[:, 0:32, :], bank(0).rearrange("p (c n) -> p c n", n=16), gbb32, ALU.add, [BK(0), 'gb'], ['Gtok'])
    tt('dve', Gtok[:, 32:34, :], bank(1)[:, 0:32].rearrange("p (c n) -> p c n", n=16), gbb2, ALU.add,
       [BK(1), 'gb'], ['Gtok'])
    act(tE, Gtok[:, :, 8:16], AF.Exp, ['Gtok'], ['tE'], scale=-1.0)
    act(Lt, tE, AF.Ln, ['tE'], ['Lt'], bias=1.0)
    for c in range(NCH):
        bk = 2 if c < 17 else 3
        o = bank(bk)[:, (c % 17) * 16:(c % 17) * 16 + 16]
        mmg(o[:, 0:4], [(SU, Lt[:, c, 0:4])], ['Lt', 'cf'], [BK(bk)])
        mmg(o[:, 4:8], [(SL, Lt[:, c, 4:8])], ['Lt', 'cf'], [BK(bk)])
        mmg(o[:, 8:16], [(ones, Lt[:, c, 0:8])], ['Lt', 'cf'], [BK(bk)])
    for (c0, c1, bk) in ((0, 17, 2), (17, 34, 3)):
        rp = bank(bk)[:, 0:17 * 16].rearrange("p (c n) -> p c n", n=16)
        tt('dve', tE[:, c0:c1, :], Gtok[:, c0:c1, 0:8], rp[:, :, 0:8], ALU.subtract, ['Gtok', BK(bk), 'Lt'], ['tE'])
        act(WC[:, c0:c1, :], tE[:, c0:c1, :], AF.Exp, ['tE'], ['WC'])
        act(LB[:, c0:c1, :], rp[:, :, 0:8], AF.Exp, [BK(bk)], ['LB'], scale=-1.0)
        act(PHI[:, c0:c1, :], rp[:, :, 8:16], AF.Exp, [BK(bk)], ['PHI'], scale=-1.0)
    act(tR, lgb, AF.Exp, ['lgb'], ['tR'], scale=-1.0)
    act(tR2, tR, AF.Ln, ['tR'], ['tR2'], bias=1.0)
    tt('dve', tR, posw, tR2, ALU.mult, ['tR2', 'cf', 'tR'], ['tR'])
    act(WCR, tR, AF.Exp, ['tR'], ['WCR'], scale=-1.0)
    act(RFR, tR, AF.Exp, ['tR'], ['RFR'])
    act(PHIR, tR2, AF.Exp, ['tR2'], ['PHIR'], scale=-128.0)

    wub = [A.alloc([8, 768], BF16) for _ in range(2)]
    QT = A.alloc([TA], BF16)
    KT = A.alloc([TA], BF16)
    Vext = A.alloc([NCH, 129], F32)
    Ktok = A.alloc([NCH, 128], BF16)
    OGs = A.alloc([NL, 128], BF16)
    Hs = A.alloc([NL, 128], F32)
    yv = [A.alloc([128], F32) for _ in range(2)]
    ya = [A.alloc([128], F32) for _ in range(2)]
    yb = [A.alloc([128], F32) for _ in range(2)]
    cosT = [A.alloc([512], F32) for _ in range(2)]
    sinT = [A.alloc([512], F32) for _ in range(2)]
    r1 = [A.alloc([512], F32) for _ in range(1)] * 2
    r2 = [A.alloc([512], F32) for _ in range(1)] * 2
    Ce = [A.alloc([129], F32) for _ in range(2)]
    Cd = [A.alloc([129], F32) for _ in range(2)]
    Cdb = [A.alloc([129], BF16) for _ in range(2)]
    Vw = [[A.alloc([129], BF16) for _ in range(2)] for _ in range(2)]
    Sm = [[A.alloc([128], BF16) for _ in range(2)] for _ in range(2)]
    denA = A.alloc([2, NCH], F32)
    recA = A.alloc([2, NCH], F32)
    ssq = A.alloc([NL], F32)
    sdq = A.alloc([NL], F32)
    rsq = A.alloc([NL], F32)
    ty = [A.alloc([128], F32) for _ in range(2)]
    memset('dve', Vext[:, :, 128:129], 1.0, ['Vext'])

    def load_wu(u):
        j = u % 2
        S.op('pool', lambda e: e.dma_start(out=wub[j], in_=winh[u].rearrange("(k p) n -> p k n", p=128)),
             (), ['wub%d' % j], dma=True)

    load_wu(0)
    order = [list(range(NCH)), [1, 0] + list(range(NCH - 1, 1, -1))]
    NBLK = (TA + 511) // 512
    for u in range(NU):
        mL = u < NU // 2
        wj = u % 2
        w = wub[wj]
        wk = 'wub%d' % wj
        if u + 1 < NU:
            load_wu(u + 1)
        for blk in range(NBLK):
            t0 = blk * 512
            n = min(512, TA - t0)
            j = blk % 2
            rhs = [hT[:, k, t0:t0 + n] for k in range(8)]
            if mL:
                bq, bk_ = j, 2 + j
                mmg(bank(bq)[:, 0:n], [(w[:, k, 0:128], rhs[k]) for k in range(8)], ['hT', wk], [BK(bq)])
                mmg(bank(bk_)[:, 0:n], [(w[:, k, 128:256], rhs[k]) for k in range(8)], ['hT', wk], [BK(bk_)])
                cp('act', QT[:, t0:t0 + n], bank(bq)[:, 0:n], [BK(bq)], ['QT'])
                act(KT[:, t0:t0 + n], bank(bk_)[:, 0:n], AF.Copy, [BK(bk_)], ['KT'], scale=KS)
            else:
                dma('sp', cosT[j][:, 0:n], rope[0][:, t0:t0 + n], (), ['cos%d' % j])
                dma('sp', sinT[j][:, 0:n], rope[1][:, t0:t0 + n], (), ['sin%d' % j])
                for (dst, dk, c0, sc) in ((QT, 'QT', 0, 1.0), (KT, 'KT', 128, KS)):
                    b0, b1 = (0, 1) if c0 == 0 else (2, 3)
                    mmg(bank(b0)[:, 0:n], [(w[:, k, c0:c0 + 128], rhs[k]) for k in range(8)], ['hT', wk], [BK(b0)])
                    mmg(bank(b1)[:, 0:n], [(w[:, k, 512 + c0:640 + c0], rhs[k]) for k in range(8)], ['hT', wk],
                        [BK(b1)])
                    stt(r1[j][:, 0:n], bank(b0)[:, 0:n], sc, cosT[j][:, 0:n], ALU.mult, ALU.mult,
                        [BK(b0), 'cos%d' % j], ['r1'])
                    stt(r2[j][:, 0:n], bank(b1)[:, 0:n], sc, sinT[j][:, 0:n], ALU.mult, ALU.mult,
                        [BK(b1), 'sin%d' % j], ['r2'])
                    tt('pool', dst[:, t0:t0 + n], r1[j][:, 0:n], r2[j][:, 0:n], ALU.add, ['r1', 'r2'],
                       [dk])
        for c in range(NCH):
            b = 4 + (c % 2)
            mmg(bank(b)[:, 0:256], [(hT[:, k, c * 128:(c + 1) * 128], w[:, k, 256:512]) for k in range(8)],
                ['hT', wk], [BK(b)])
            cp('act', Vext[:, c, 0:128], bank(b)[:, 0:128], [BK(b)], ['Vext'])
            if c >= 2:
                act(OGs[:, c - 2, :], bank(b)[:, 128:256], AF.Sigmoid if mL else AF.Silu, [BK(b)], ['OGs'])
        for c0 in range(0, NCH, 8):
            cs = list(range(c0, min(c0 + 8, NCH)))
            b = 6 + ((c0 // 8) % 2)
            trg([(bankbf(b)[:, i * 128:(i + 1) * 128], KT[:, c * 128:(c + 1) * 128], identb)
                 for i, c in enumerate(cs)], ['KT', 'identb'], [BK(b)])
            cp('act', Ktok[:, c0:c0 + len(cs), :],
               bankbf(b)[:, 0:len(cs) * 128].rearrange("p (c n) -> p c n", n=128), [BK(b)], ['Ktok'])
        for d in range(2):
            memset('dve', Ce[d], 0.0, ['Ce%d' % d])
        gk = ['PHI', 'WC', 'LB'] if mL else ['PHIR', 'WCR', 'RFR']

        def emit_out(d, c):
            L = c - 2
            jg = d * 4 + (u % 2)
            bO = 3 * d + 1
            first = (d == 0) == (L <= 15)
            hk = 'Hs%d' % L
            if mL:
                dn = denA[:, d, c:c + 1]
                rc = recA[:, d, c:c + 1]
                dk = 'den%d_%d' % (d, c)
                act(dn, bank(bO)[:, 128:129], AF.Abs, [BK(bO)], [dk])
                tt('dve', dn, dn, LB[:, c, jg:jg + 1], ALU.max, [dk] + gk, [dk])
                recip(rc, dn, [dk], ['r' + dk])
                scal = rc
                sck = ['r' + dk]
            else:
                scal = RFR[:, jg:jg + 1]
                sck = gk
            if first:
                ts('dve', Hs[:, L, :], bank(bO)[:, 0:128], scal, ALU.mult, [BK(bO)] + sck, [hk])
            else:
                stt(Hs[:, L, :], bank(bO)[:, 0:128], scal, Hs[:, L, :], ALU.mult, ALU.add,
                    [BK(bO), hk] + sck, [hk])

        def emit_vw_u(d, step):
            c = order[d][step]
            jg = d * 4 + (u % 2)
            wc = WC[:, c, jg:jg + 1] if mL else WCR[:, jg:jg + 1]
            vj = step % 2
            ts('dve', Vw[d][vj], Vext[:, c, :], wc, ALU.mult, ['Vext'] + gk, ['Vw%d%d' % (d, vj)])
            mmg(bank(3 * d + 2)[:, 0:129], [(Ktok[:, c, :], Vw[d][vj])], ['Ktok', 'Vw%d%d' % (d, vj)],
                [BK(3 * d + 2)])

        pending = [None, None]
        for d in range(2):
            emit_vw_u(d, 0)
        for step in range(NCH):
            for d in range(2):
                c = order[d][step]
                jg = d * 4 + (u % 2)
                phi = PHI[:, c, jg:jg + 1] if mL else PHIR[:, jg:jg + 1]
                vj = step % 2
                vw = Vw[d][vj]
                vk = 'Vw%d%d' % (d, vj)
                bS, bO, bU = 3 * d, 3 * d + 1, 3 * d + 2
                tok = slice(c * 128, (c + 1) * 128)
                if c >= 2:
                    mmg(bank(bS)[:, 0:128], [(KT[:, tok], QT[:, tok])], ['KT', 'QT'], [BK(bS)])
                ts('dve', Cd[d], Ce[d], phi, ALU.mult, ['Ce%d' % d] + gk, ['Cd%d' % d])
                if c >= 2:
                    cp('act', Cdb[d], Cd[d], ['Cd%d' % d], ['Cdb%d' % d])
                    sm = Sm[d][vj]
                    sk_ = 'Sm%d%d' % (d, vj)
                    tt('dve', sm, bank(bS)[:, 0:128], maskF if d == 0 else maskB, ALU.mult, [BK(bS), 'cf'], [sk_])
                tt('dve', Ce[d], Cd[d], bank(bU)[:, 0:129], ALU.add, ['Cd%d' % d, BK(bU)], ['Ce%d' % d])
                if step + 1 < NCH:
                    emit_vw_u(d, step + 1)
                if pending[d] is not None:
                    emit_out(d, pending[d])
                    pending[d] = None
                if c >= 2:
                    mmg(bank(bO)[:, 0:129], [(sm, vw), (QT[:, tok], Cdb[d])], [sk_, vk, 'QT', 'Cdb%d' % d],
                        [BK(bO)])
                    pending[d] = c
        for d in range(2):
            if pending[d] is not None:
                emit_out(d, pending[d])
        for L in range(NL):
            act(junk[:, 0:128], Hs[:, L, :], AF.Square, ['Hs%d' % L], ['junk', 'ssq%d' % L], accum=ssq[:, L:L + 1])
        act(sdq, ssq, AF.Sqrt, ['ssq%d' % L for L in range(NL)], ['sdq'], bias=EPS, scale=1.0 / 128)
        recip(rsq, sdq, ['sdq'], ['rsq'])
        for L in range(NL):
            j = L % 2
            stt(ty[j], Hs[:, L, :], rsq[:, L:L + 1], hnwb[:, u * 128:(u + 1) * 128], ALU.mult, ALU.mult,
                ['Hs%d' % L, 'rsq', 'hnwb'], ['ty%d' % j])
            tt('dve', yv[j], ty[j], OGs[:, L, :], ALU.mult, ['ty%d' % j, 'OGs'], ['yv%d' % j])
            ts('dve', ya[j], yv[j], yfl[:, 0:1], ALU.mult, ['yv%d' % j, 'yfl'], ['ya%d' % j])
            ts('dve', yb[j], yv[j], yfl[:, 1:2], ALU.mult, ['yv%d' % j, 'yfl'], ['yb%d' % j])
            dma('sp', y_full[L * 128:(L + 1) * 128, u * 128:(u + 1) * 128], ya[j], ['ya%d' % j],
                ['yfa%d_%d' % (u, L)])
            dma('sp', y_full[L * 128:(L + 1) * 128, 512 + u * 128:512 + (u + 1) * 128], yb[j], ['yb%d' % j],
                ['yfb%d_%d' % (u, L)])
    S.barrier()
    for q in range(4):
        S.op('pool', lambda e, q=q: e.collective_compute(
            "AllReduce", ALU.add, replica_groups=RG, ins=[y_full[q * 1024:(q + 1) * 1024, :].opt()],
            outs=[y_ar[q * 1024:(q + 1) * 1024, :].opt()]), (), ['y_ar%d' % q], cc=True)
    S.barrier()
    if stop_after == 'C':
        return _finish(nc, S, es, dbg, dbg_out, None)

    A.top = (mark0 + 63) // 64 * 64
    H2X = A.alloc([NL, ROWW], BF16)
    AFF = A.alloc([NL, 16], F32)
    SLOT = A.alloc([NL, 16], I32)
    markD = A.top
    woutb = A.alloc([8, D], BF16)
    S.op('pool', lambda e: e.dma_start(out=woutb, in_=w_out.rearrange("(k p) n -> p k n", p=128)), (), ['woutb'],
         dma=True)
    wr = A.alloc([8, 16], F32)
    dma('sp', wr, w_router.rearrange("(k p) n -> p k n", p=128), (), ['wr'])
    G1 = A.alloc([D], F32)
    W2 = A.alloc([D], F32)
    B2 = A.alloc([D], F32)
    dma('sp', G1, mod_scr[:, 2 * D:3 * D], ['mod_scr'], ['G1'])
    dma('sp', W2, mod_scr[:, 4 * D:5 * D], ['mod_scr'], ['W2'])
    dma('sp', B2, mod_scr[:, 3 * D:4 * D], ['mod_scr'], ['B2'])
    ND = 3
    yt = [A.alloc([D], BF16) for _ in range(ND)]
    ytT = [A.alloc([8, 128], BF16) for _ in range(ND)]
    xt = [A.alloc([D], F32) for _ in range(ND)]
    tg = [A.alloc([D], F32) for _ in range(ND)]
    xl = [A.alloc([D], F32) for _ in range(ND)]
    xs = [A.alloc([D], F32) for _ in range(ND)]
    h2f = [A.alloc([D], F32) for _ in range(ND)]
    h2T = [A.alloc([8, 128], F32) for _ in range(ND)]
    junk = A.alloc([D], BF16)
    ss2 = A.alloc([NL], F32)
    sd2 = A.alloc([NL], F32)
    rs2 = A.alloc([NL], F32)
    mxA = A.alloc([NL], F32)
    seA = A.alloc([NL], F32)
    rseA = A.alloc([NL], F32)
    ex = [A.alloc([16], F32) for _ in range(ND)]
    for L in range(NL):
        j = L % ND
        jb = L % 2
        S.op('pool', lambda e, L=L, j=j: e.dma_start(out=yt[j], in_=y_ar[L * 128:(L + 1) * 128, :]), (),
             ['yt%d' % j], dma=True)
        dma('sp', xt[j], xa[TC + L * 128:TC + (L + 1) * 128, :], (), ['xt%d' % j])
        trg([(bankbf(jb)[:, k * 128:(k + 1) * 128], yt[j][:, k * 128:(k + 1) * 128], identb) for k in range(8)],
            ['yt%d' % j, 'identb'], [BK(jb)])
        cp('act', ytT[j], bankbf(jb).rearrange("p (k t) -> p k t", k=8), [BK(jb)], ['ytT%d' % j])
        for h in range(2):
            b = 2 + h
            mmg(bank(b), [(ytT[j][:, k, :], woutb[:, k, h * 512:(h + 1) * 512]) for k in range(8)],
                ['ytT%d' % j, 'woutb'], [BK(b)])
            tt('dve', tg[j][:, h * 512:(h + 1) * 512], bank(b), G1[:, h * 512:(h + 1) * 512], ALU.mult,
               [BK(b), 'G1'], ['tg%d' % j])
        tt('dve', xl[j], tg[j], xt[j], ALU.add, ['tg%d' % j, 'xt%d' % j], ['xl%d' % j])
        act(xs[j], xl[j], AF.Copy, ['xl%d' % j], ['xs%d' % j], scale=0.5 if use_ar else 1.0)
        dma('sp', xl_scr[L * 128:(L + 1) * 128, :], xs[j], ['xs%d' % j], ['xls%d' % L])
        act(junk, xl[j], AF.Square, ['xl%d' % j], ['junk', 'ss2%d' % L], accum=ss2[:, L:L + 1])
        act(sd2[:, L:L + 1], ss2[:, L:L + 1], AF.Sqrt, ['ss2%d' % L], ['sd2%d' % L], bias=EPS, scale=1.0 / D)
        recip(rs2[:, L:L + 1], sd2[:, L:L + 1], ['sd2%d' % L], ['rs2%d' % L])
        stt(tg[j], xl[j], rs2[:, L:L + 1], W2, ALU.mult, ALU.mult, ['xl%d' % j, 'rs2%d' % L, 'W2', 'tg%d' % j],
            ['tg%d' % j])
        tt('dve', h2f[j], tg[j], B2, ALU.add, ['tg%d' % j, 'B2'], ['h2f%d' % j])
        cp('act', H2X[:, L, 0:D], h2f[j], ['h2f%d' % j], ['H2Xa%d' % L])
        for h in range(2):
            b = 4 + h
            trg([(bank(b)[:, i * 128:(i + 1) * 128], h2f[j][:, (4 * h + i) * 128:(4 * h + i + 1) * 128], ident)
                 for i in range(4)], ['h2f%d' % j, 'cf'], [BK(b)])
            cp('act', h2T[j][:, 4 * h:4 * h + 4, :], bank(b).rearrange("p (k t) -> p k t", k=4), [BK(b)],
               ['h2T%d' % j])
        b = 6 + jb
        mmg(bank(b)[:, 0:16], [(h2T[j][:, k, :], wr[:, k, :]) for k in range(8)], ['h2T%d' % j, 'wr'], [BK(b)])
        S.op('dve', lambda e, o=mxA[:, L:L + 1], i_=bank(b)[:, 0:16]: e.tensor_reduce(out=o, in_=i_, axis=AX.X,
                                                                                     op=ALU.max, negate=True),
             [BK(b)], ['mx%d' % L])
        act(ex[j], bank(b)[:, 0:16], AF.Exp, [BK(b), 'mx%d' % L], ['ex%d' % j, 'se%d' % L], bias=mxA[:, L:L + 1],
            accum=seA[:, L:L + 1])
        recip(rseA[:, L:L + 1], seA[:, L:L + 1], ['se%d' % L], ['rse%d' % L])
        ts('dve', AFF[:, L, :], ex[j], rseA[:, L:L + 1], ALU.mult, ['ex%d' % j, 'rse%d' % L], ['AFF%d' % L])
        cp('dve', H2X[:, L, D:D + 32].bitcast(F32), AFF[:, L, :], ['AFF%d' % L], ['H2Xb%d' % L])
        cp('dve', H2X[:, L, D + 32:D + 34].bitcast(I32), ciT[:, L:L + 1], ['ci'], ['H2Xc%d' % L])
    S.barrier()
    if stop_after == 'D':
        return _finish(nc, S, es, dbg, dbg_out, None)

    A.top = markD
    affT = A.alloc([T], F32)
    msk = A.alloc([T], F32)
    pos = A.alloc([T], F32)
    one16 = A.alloc([T], F32)
    sc16 = A.alloc([16], F32)
    lo, hi, sm_, mid, cnt, ge, u1, u2 = [sc16[0:16, i:i + 1] for i in range(8)]
    for g in range(8):
        b = g % 2
        trg([(bank(b)[0:16, i * 128:(i + 1) * 128], AFF[:, 4 * g + i, :], ident) for i in range(4)], ['AFF', 'cf'],
            [BK(b)])
        cp('act', affT[0:16, g * 512:(g + 1) * 512], bank(b)[0:16, :], [BK(b)], ['affT'])
    memset('dve', sc16[0:16, :], 0.0, ['sc16'])
    memset('dve', hi, 1.0, ['sc16'])
    memset('dve', one16[0:16, :], 1.0, ['one16'])
    for it in range(NIT):
        tt('dve', sm_, lo, hi, ALU.add, ['sc16'], ['sc16'])
        ts('dve', mid, sm_, 0.5, ALU.mult, ['sc16'], ['sc16'])
        ts('dve', msk[0:16, :], affT[0:16, :], mid, ALU.is_ge, ['affT', 'sc16'], ['msk', 'sc16'], s2=None,
           op1=ALU.add, accum=cnt)
        ts('dve', ge, cnt, CAP - 0.5, ALU.is_ge, ['sc16'], ['sc16'])
        tt('dve', u1, ge, mid, ALU.mult, ['sc16'], ['sc16'])
        tt('dve', lo, lo, u1, ALU.max, ['sc16'], ['sc16'])
        stt(u2, ge, 4.0, mid, ALU.mult, ALU.add, ['sc16'], ['sc16'])
        tt('dve', hi, hi, u2, ALU.min, ['sc16'], ['sc16'])
    ts('dve', msk[0:16, :], affT[0:16, :], lo, ALU.is_ge, ['affT', 'sc16'], ['msk'])
    S.op('dve', lambda e: e.tensor_tensor_scan(out=pos[0:16, :], data0=one16[0:16, :], data1=msk[0:16, :],
                                               initial=0.0, op0=ALU.mult, op1=ALU.add), ['msk', 'one16'], ['pos'])
    stt(pos[0:16, :], pos[0:16, :], -1001.0, msk[0:16, :], ALU.add, ALU.mult, ['pos', 'msk'], ['pos'])
    ts('dve', pos[0:16, :], pos[0:16, :], 1000.0, ALU.add, ['pos'], ['pos'])
    trg([(bank(2)[:, L * 16:(L + 1) * 16], pos[0:16, L * 128:(L + 1) * 128], ident[0:16, 0:16]) for L in range(NL)],
        ['pos', 'cf'], [BK(2)])
    cp('dve', SLOT, bank(2).rearrange("p (c n) -> p c n", n=16), [BK(2)], ['SLOT'])
    S.barrier()
    if stop_after == 'E':
        return _finish(nc, S, es, dbg, dbg_out, None)

    A.top = markD
    G2 = A.alloc([D], F32)
    dma('sp', G2, mod_scr[:, 5 * D:6 * D], ['mod_scr'], ['G2'])
    Wd = A.alloc([NFC, D], BF16)
    NWB = 3
    Wgs = [A.alloc([8, 256], BF16) for _ in range(NWB)]
    Wus = [A.alloc([8, 256], BF16) for _ in range(NWB)]
    actb = A.alloc([NFC, CAP], BF16)
    XG = [A.alloc([4, ROWW], BF16) for _ in range(2)]
    xgT = [A.alloc([8, CAP], BF16) for _ in range(1)]
    yo = [A.alloc([D], F32) for _ in range(2)]
    sa = [A.alloc([CAP], BF16) for _ in range(2)]
    regs = {}

    def bcreg(e):
        if 'bc' not in regs:
            regs['bc'] = e.alloc_register('bc')
            e.reg_mov(regs['bc'], CAP - 1)
        return regs['bc']

    prev_add = []
    wcount = 0
    ycount = 0

    def scatter_rows(e_, Ls):
        for L in Ls:
            S.op('pool', lambda e, L=L, e_=e_: e.indirect_dma_start(
                out=xg_flat, out_offset=bass.IndirectOffsetOnAxis(ap=SLOT[:, L, e_:e_ + 1], axis=0),
                in_=H2X[:, L, :], in_offset=None, element_offset=e_ * CAP * ROWW, bounds_check=bcreg(e),
                oob_is_err=False),
                ['H2X', 'SLOT'], ['xgs%d_%d' % (e_, L)], dma=True)

    def load_wd(e_):
        for hh in range(2):
            S.op('pool', lambda e, hh=hh, e_=e_: e.dma_start(
                out=Wd[:, hh * 11:(hh + 1) * 11, :],
                in_=w_down[e_][hh * 1408:(hh + 1) * 1408, :].rearrange("(f p) n -> p f n", p=128)),
                (), ['Wd'], dma=True)

    scatter_rows(0, range(NL))
    for e_ in range(NEL):
        xj = e_ % 2
        dma('sp', XG[xj], xg_scr[e_].rearrange("(s p) n -> p s n", p=128), ['xgs%d_%d' % (e_, L) for L in range(NL)],
            ['XG%d' % xj])
        for k0 in range(0, 8, 2):
            b = 6 + ((k0 // 2) % 2)
            trg([(bankbf(b)[:, (kk * 4 + s_) * 128:(kk * 4 + s_ + 1) * 128],
                  XG[xj][:, s_, (k0 + kk) * 128:(k0 + kk + 1) * 128], identb) for kk in range(2) for s_ in range(4)],
                ['XG%d' % xj, 'identb'], [BK(b)])
            cp('act', xgT[0][:, k0:k0 + 2, :], bankbf(b).rearrange("p (k t) -> p k t", k=2), [BK(b)],
               ['xgT0'])
        for fb in range(NFC // 2):
            wj = wcount % NWB
            wcount += 1
            S.op('pool', lambda e, fb=fb, e_=e_, wj=wj: e.dma_start(
                out=Wgs[wj], in_=w_gate[e_][:, fb * 256:(fb + 1) * 256].rearrange("(k p) n -> p k n", p=128)),
                (), ['Wgs%d' % wj], dma=True)
            S.op('pool', lambda e, fb=fb, e_=e_, wj=wj: e.dma_start(
                out=Wus[wj], in_=w_up[e_][:, fb * 256:(fb + 1) * 256].rearrange("(k p) n -> p k n", p=128)),
                (), ['Wus%d' % wj], dma=True)
            if fb == 2:
                load_wd(e_)
            if e_ + 1 < NEL:
                scatter_rows(e_ + 1, range(fb * 3, min(NL, fb * 3 + 3)))
            for fc in range(2):
                fi = fb * 2 + fc
                j = fi % 2
                mmg(bank(j), [(Wgs[wj][:, k, fc * 128:(fc + 1) * 128], xgT[0][:, k, :]) for k in range(8)],
                    ['Wgs%d' % wj, 'xgT0'], [BK(j)])
                mmg(bank(2 + j), [(Wus[wj][:, k, fc * 128:(fc + 1) * 128], xgT[0][:, k, :]) for k in range(8)],
                    ['Wus%d' % wj, 'xgT0'], [BK(2 + j)])
                act(sa[j], bank(j), AF.Silu, [BK(j)], ['sa%d' % j])
                tt('dve', actb[:, fi, :], sa[j], bank(2 + j), ALU.mult, ['sa%d' % j, BK(2 + j)], ['actb'])
        cur_add = []
        for s_ in range(4):
            yj = ycount % 2
            ycount += 1
            gcol = XG[xj][:, s_, D:D + 32].bitcast(F32)[:, e_:e_ + 1]
            icol = XG[xj][:, s_, D + 32:D + 34].bitcast(I32)
            for h in range(2):
                b = 4 + h
                mmg(bank(b), [(actb[:, f, s_ * 128:(s_ + 1) * 128], Wd[:, f, h * 512:(h + 1) * 512]) for f in range(NFC)],
                    ['actb', 'Wd'], [BK(b)])
                stt(yo[yj][:, h * 512:(h + 1) * 512], bank(b), gcol, G2[:, h * 512:(h + 1) * 512], ALU.mult, ALU.mult,
                    [BK(b), 'XG%d' % xj, 'G2'], ['yo%d' % yj])
            m = S.op('pool', lambda e, yj=yj, icol=icol: e.indirect_dma_start(
                out=xl_scr, out_offset=bass.IndirectOffsetOnAxis(ap=icol, axis=0), in_=yo[yj], in_offset=None,
                compute_op=ALU.add), ['yo%d' % yj, 'XG%d' % xj, 'xl_scr'], ['add%d_%d' % (e_, s_)], dma=True,
                extra=prev_add)
            cur_add.append(m)
        prev_add = cur_add
    S.barrier()
    if use_ar:
        for q in range(4):
            S.op('pool', lambda e, q=q: e.collective_compute(
                "AllReduce", ALU.add, replica_groups=RG, ins=[xl_scr[q * 1024:(q + 1) * 1024, :].opt()],
                outs=[xl_ar[q * 1024:(q + 1) * 1024, :].opt()]), (), ['xl_ar%d' % q], cc=True)
        S.barrier()

    A.top = markD
    nw3 = A.alloc([D], F32)
    dma('sp', nw3, nw[2].partition_broadcast(128), (), ['nw3'])
    xf = [A.alloc([D], F32) for _ in range(2)]
    of = [A.alloc([D], F32) for _ in range(2)]
    junk = A.alloc([D], BF16)
    ss3 = A.alloc([NL], F32)
    sd3 = A.alloc([NL], F32)
    rs3 = A.alloc([NL], F32)
    for L in range(NL):
        j = L % 2
        dma('sp', xf[j], xl_ar[L * 128:(L + 1) * 128, :], (), ['xf%d' % j])
        act(junk, xf[j], AF.Square, ['xf%d' % j], ['junk', 'ss3%d' % L], accum=ss3[:, L:L + 1])
        act(sd3[:, L:L + 1], ss3[:, L:L + 1], AF.Sqrt, ['ss3%d' % L], ['sd3%d' % L], bias=EPS, scale=1.0 / D)
        recip(rs3[:, L:L + 1], sd3[:, L:L + 1], ['sd3%d' % L], ['rs3%d' % L])
        stt(of[j], xf[j], rs3[:, L:L + 1], nw3, ALU.mult, ALU.mult, ['xf%d' % j, 'rs3%d' % L, 'nw3'], ['of%d' % j])
        dma('sp', out[L * 128:(L + 1) * 128, :], of[j], ['of%d' % j], ['out%d' % L])
    return _finish(nc, S, es, dbg, dbg_out, None)


def _end_of(ap, arena_t):
    return int(ap.offset - arena_t[:, 0:1].offset) + int(np.prod(ap.shape[1:])) * DSZ[ap.dtype]


def _finish(nc, S, es, dbg, dbg_out, _unused):
    if dbg is not None:
        S.op('sp', lambda e: e.dma_start(out=dbg_out, in_=dbg[0]()), (), ['dbg'], dma=True)
    S.barrier()
    S.emit()
    es.close()
    return nc


def _consts():
    p = np.arange(128)
    cf = np.zeros((128, CFW), np.float32)
    cf[:, 0:128] = np.eye(128)
    cf[:, 128:256] = 1.0
    cf[:, 256:384] = (p[:, None] <= p[None, :])
    cf[:, 384:512] = (p[:, None] >= p[None, :])
    cf[:, 512:640] = (p[:, None] > p[None, :])
    cf[:, 640:768] = (p[:, None] < p[None, :])
    cf[:, 768:772] = (127 - p)[:, None]
    cf[:, 772:776] = p[:, None]
    ci = (np.arange(NL)[None, :] * 128 + p[:, None]).astype(np.int32)
    t = np.arange(T)
    rows = (t // 64).astype(np.float32)
    cols = (t % 64).astype(np.float32)
    inv = (np.float32(10000.0) ** (-np.arange(32, dtype=np.float32) / np.float32(32))).astype(np.float32)
    ar = (rows[:, None] * inv[None, :]).astype(np.float32)
    ac = (cols[:, None] * inv[None, :]).astype(np.float32)
    C = np.ones((128, TA), np.float32)
    Sn = np.zeros((128, TA), np.float32)
    for i in range(128):
        ang = ar[:, i % 32] if i < 64 else ac[:, i % 32]
        C[i, TC:] = np.cos(ang)
        Sn[i, TC:] = np.sin(ang) * (-1.0 if (i % 64) < 32 else 1.0)
    rope = np.stack([C, Sn]).astype(np.float32)
    perm = np.array([i + 32 if (i % 64) < 32 else i - 32 for i in range(128)])
    return cf, ci, rope, perm


def _prep(inputs, NEL, ncores):
    f = lambda a: np.ascontiguousarray(np.asarray(a, dtype=np.float32))
    x, c, ctx, c_ctx = f(inputs['x']), f(inputs['c']), f(inputs['ctx']), f(inputs['c_ctx'])
    w_in = f(inputs['w_in'])[0]
    cf, ci, rope, perm = _consts()
    winh = np.zeros((8, D, 768), np.float32)
    for u in range(8):
        if u < 4:
            base = [0, 512, 1024, 1536]
            h = u
        else:
            base = [2064, 2576, 3088, 3600]
            h = u - 4
        for i, b in enumerate(base):
            winh[u, :, i * 128:(i + 1) * 128] = w_in[:, b + h * 128:b + (h + 1) * 128]
        if u >= 4:
            winh[u, :, 512:640] = winh[u, :, 0:128][:, perm]
            winh[u, :, 640:768] = winh[u, :, 128:256][:, perm]
    wg_all = np.ascontiguousarray(w_in[:, 2048:2064])
    gb_all = f(inputs['mlstm_gate_bias'])[0]
    dl_all = f(inputs['ret_decay_logit'])[0].reshape(8)
    mnw, rnw = f(inputs['mlstm_norm_w'])[0], f(inputs['ret_norm_w'])[0]
    w_out_full = f(inputs['w_out'])[0]
    rows = []
    for gg in range(2):
        for h in (2 * gg, 2 * gg + 1):
            rows += list(range(h * 128, (h + 1) * 128))
        for h in (2 * gg, 2 * gg + 1):
            rows += list(range(512 + h * 128, 512 + (h + 1) * 128))
    shared = dict(
        w_ada=f(inputs['w_ada'])[0], b_ada=f(inputs['b_ada'])[0][None],
        nw=np.stack([f(inputs['norm1_w'])[0], f(inputs['norm2_w'])[0], f(inputs['final_norm_w'])]),
        w_out=np.ascontiguousarray(w_out_full[rows, :]), rope=rope, cf=cf, ci=ci)
    percore = []
    for g in range(2):
        hl = [2 * g, 2 * g + 1]
        hp = hl + [h for h in range(4) if h not in hl]
        gcols = [t * 4 + h for t in range(4) for h in hp]
        dcols = [dr * 4 + h for dr in range(2) for h in hp]
        percore.append(dict(
            winh=np.ascontiguousarray(winh[[hl[0], hl[1], 4 + hl[0], 4 + hl[1]]]),
            wg=np.ascontiguousarray(wg_all[:, gcols]), gbias=np.ascontiguousarray(gb_all[gcols])[None],
            dlog=np.ascontiguousarray(dl_all[dcols])[None],
            hnw=np.concatenate([mnw[hl[0] * 128:(hl[1] + 1) * 128], rnw[hl[0] * 128:(hl[1] + 1) * 128]])[None],
            yflag=np.array([[1.0, 0.0]] if g == 0 else [[0.0, 1.0]], np.float32)))
    wr = f(inputs['w_router'])[0]
    wgate, wup, wdown = f(inputs['w_gate'])[0], f(inputs['w_up'])[0], f(inputs['w_down'])[0]
    maps = []
    ngrp = NE // NEL
    for core in range(ncores):
        b = core // ngrp
        g = core % ngrp
        eidx = list(range(g * NEL, (g + 1) * NEL)) + [e for e in range(NE) if not (g * NEL <= e < (g + 1) * NEL)]
        m = dict(shared)
        m.update(percore[g])
        m['xa'] = np.concatenate([ctx[b], x[b]], axis=0)
        cv = np.zeros((128, 16), np.float32)
        cv[:, 0:8] = c[b].reshape(8, 128).T
        cv[:, 8:16] = c_ctx.reshape(8, 128).T
        m['cvec'] = cv
        m['w_router'] = np.ascontiguousarray(wr[:, eidx])
        m['w_gate'] = np.ascontiguousarray(wgate[g * NEL:(g + 1) * NEL])
        m['w_up'] = np.ascontiguousarray(wup[g * NEL:(g + 1) * NEL])
        m['w_down'] = np.ascontiguousarray(wdown[g * NEL:(g + 1) * NEL])
        m['flag'] = np.array([[1.0 if g == 0 else 0.0]], np.float32)
        maps.append(m)
    return maps


NEL_DEFAULT = 8
NCORES = 8


def kernel(**inputs):
    ngrp = NE // NEL_DEFAULT
    nc = build_nc(NEL_DEFAULT, use_ar=(ngrp > 1))
    maps = _prep(inputs, NEL_DEFAULT, NCORES)
    res = run_bass_kernel_spmd(nc, maps, core_ids=list(range(NCORES)))
    outs = [np.asarray(res.results[b * ngrp]["out"], dtype=np.float32) for b in range(4)]
    return np.stack(outs, axis=0)
```

```python
import numpy as np
import ml_dtypes
from contextlib import ExitStack
import concourse.bass as bass
import concourse.mybir as mybir
from concourse.bass_utils import run_bass_kernel_spmd

F32 = mybir.dt.float32
BF16 = mybir.dt.bfloat16
I32 = mybir.dt.int32
U8 = mybir.dt.uint8
AF = mybir.ActivationFunctionType
ALU = mybir.AluOpType
AX = mybir.AxisListType
DSZ = {F32: 4, BF16: 2, I32: 4, U8: 1}

D = 1024
T = 4096
TC = 256
TA = T + TC
NCH = TA // 128
NL = T // 128
FF = 2816
NFC = FF // 128
NE = 16
CAP = 512
EPS = 1e-6
KS = 128 ** -0.5
ROWW = 1058
CFW = 6 * 128 + 8
NIT = 36
NU = 4
RG = [[0, 1], [2, 3], [4, 5], [6, 7]]


class Arena:
    def __init__(self, ap, size):
        self.ap, self.size, self.top = ap, size, 0

    def alloc(self, shape, dt):
        n = int(np.prod(shape)) * DSZ[dt]
        off = (self.top + 63) // 64 * 64
        assert off + n <= self.size, ("SBUF arena overflow", off + n, self.size)
        self.top = off + n
        v = self.ap[:, off:off + n].bitcast(dt)
        if len(shape) == 2:
            v = v.rearrange("p (a b) -> p a b", a=shape[0])
        elif len(shape) == 3:
            v = v.rearrange("p (a b c) -> p a b c", a=shape[0], b=shape[1])
        return v


class Sched:
    ENG = ['pe', 'dve', 'act', 'pool', 'sp']

    def __init__(self, nc, es, ndma=48):
        self.nc = nc
        self.sem = {e: es.enter_context(nc.semaphore('s_' + e)) for e in self.ENG}
        self.dsem = [es.enter_context(nc.semaphore('d%d' % i)) for i in range(ndma)]
        self.dcnt = [0] * ndma
        self.dnext = 0
        self.csem = es.enter_context(nc.semaphore('s_cc'))
        self.ccnt = 0
        self.cnt = {e: 0 for e in self.ENG}
        self.streams = {e: [] for e in self.ENG}
        self.waited = {e: {} for e in self.ENG}
        self.bufs = {}

    def op(self, eng, fn, reads=(), writes=(), dma=False, extra=(), cc=False):
        deps = set(extra)
        for k in reads:
            b = self.bufs.get(k)
            if b and b[0] is not None:
                deps.add(b[0])
        for k in writes:
            b = self.bufs.get(k)
            if b:
                if b[0] is not None:
                    deps.add(b[0])
                deps.update(b[1])
        if cc:
            self.ccnt += 1
            me = (('c', 0), self.ccnt)
        elif dma:
            slot = self.dnext
            self.dnext = (self.dnext + 1) % len(self.dsem)
            prev = self.dcnt[slot]
            if prev > 0:
                deps.add((('d', slot), prev))
            self.dcnt[slot] = prev + 16
            me = (('d', slot), prev + 16)
        else:
            self.cnt[eng] += 1
            me = (('e', eng), self.cnt[eng])
        best = {}
        for (sk, v) in deps:
            if v > best.get(sk, 0):
                best[sk] = v
        w = self.waited[eng]
        waits = []
        for sk, v in best.items():
            if sk == ('e', 'pe') and eng == 'pe':
                continue
            if w.get(sk, 0) < v:
                w[sk] = v
                waits.append((sk, v))
        self.streams[eng].append((fn, waits, me))
        for k in reads:
            self.bufs.setdefault(k, [None, []])[1].append(me)
        for k in writes:
            self.bufs[k] = [me, []]
        return me

    def barrier(self):
        allv = [(('e', e), self.cnt[e]) for e in self.ENG if self.cnt[e] > 0]
        allv += [(('d', i), v) for i, v in enumerate(self.dcnt) if v > 0]
        if self.ccnt > 0:
            allv.append((('c', 0), self.ccnt))
        for eng in self.ENG:
            w = self.waited[eng]
            waits = []
            for sk, v in allv:
                if sk == ('e', eng):
                    continue
                if w.get(sk, 0) < v:
                    w[sk] = v
                    waits.append((sk, v))
            self.streams[eng].append((None, waits, None))
        self.bufs = {}

    def emit(self):
        nc = self.nc
        S = self

        def run(name, eo):
            for fn, waits, me in S.streams[name]:
                for sk, v in waits:
                    s = S.sem[sk[1]] if sk[0] == 'e' else (S.csem if sk[0] == 'c' else S.dsem[sk[1]])
                    eo.wait_ge(s, v)
                if fn is None:
                    continue
                ins = fn(eo)
                if me[0][0] == 'e':
                    ins.then_inc(S.sem[me[0][1]], 1)
                elif me[0][0] == 'c':
                    ins.then_inc(S.csem)
                else:
                    ins.then_inc(S.dsem[me[0][1]], 16)

        with nc.Block() as block:
            @block.tensor
            def _(e):
                run('pe', e)

            @block.vector
            def _(e):
                run('dve', e)

            @block.scalar
            def _(e):
                run('act', e)

            @block.gpsimd
            def _(e):
                run('pool', e)

            @block.sync
            def _(e):
                run('sp', e)


def build_nc(NEL, use_ar=False, stop_after=None, dbg=None):
    nc = bass.Bass("TRN2", target_bir_lowering=False)

    def din(name, shape, dt=F32):
        return nc.dram_tensor(name, shape, dt, kind="ExternalInput").ap()

    xa = din("xa", [TA, D])
    cvec = din("cvec", [128, 16])
    w_ada = din("w_ada", [D, 6 * D])
    b_ada = din("b_ada", [1, 6 * D])
    nw = din("nw", [3, D])
    winh = din("winh", [NU, D, 768])
    wg = din("wg", [D, 16])
    gbias = din("gbias", [1, 16])
    dlog = din("dlog", [1, 8])
    hnw = din("hnw", [1, NU * 128])
    yflag = din("yflag", [1, 2])
    w_out = din("w_out", [D, D])
    w_router = din("w_router", [D, 16])
    w_gate = din("w_gate", [NEL, D, FF])
    w_up = din("w_up", [NEL, D, FF])
    w_down = din("w_down", [NEL, FF, D])
    rope = din("rope", [2, 128, TA])
    cf = din("cf", [128, CFW])
    ci = din("ci", [128, NL], I32)
    flag = din("flag", [1, 1])
    out = nc.dram_tensor("out", [T, D], F32, kind="ExternalOutput").ap()
    y_full = nc.dram_tensor("y_full", [T, D], F32).ap()
    y_ar = nc.dram_tensor("y_ar", [T, D], F32).ap()
    xl_scr = nc.dram_tensor("xl_scr", [T, D], F32).ap()
    mod_scr = nc.dram_tensor("mod_scr", [128, 6 * D], F32, kind="Internal").ap()
    xg_scr = nc.dram_tensor("xg_scr", [NEL, CAP, ROWW], BF16, kind="Internal").ap()
    xg_flat = xg_scr.rearrange("e c n -> (e c) n")
    xl_ar = nc.dram_tensor("xl_ar", [T, D], F32).ap() if use_ar else xl_scr
    dbg_out = None
    if dbg is not None:
        dbg_out = nc.dram_tensor("dbg", [128, dbg[1]], F32, kind="ExternalOutput").ap()

    es = ExitStack()
    ARENA = 210000
    arena_t = es.enter_context(nc.sbuf_tensor("arena", [128, ARENA], U8))
    ps = es.enter_context(nc.psum_tensor("ps", [128, 4096], F32))
    S = Sched(nc, es)
    A = Arena(arena_t, ARENA)

    def bank(b):
        return ps[:, b * 512:(b + 1) * 512]

    def bankbf(b):
        return ps[:, b * 512:(b + 1) * 512].bitcast(BF16)

    def BK(b):
        return 'bank%d' % b

    def dma(q, out_, in_, reads, writes):
        return S.op(q, lambda e: e.dma_start(out=out_, in_=in_), reads, writes, dma=True)

    def tt(eng, out_, in0, in1, op, reads, writes):
        return S.op(eng, lambda e: e.tensor_tensor(out=out_, in0=in0, in1=in1, op=op), reads, writes)

    def ts(eng, out_, in0, s1, op0, reads, writes, s2=None, op1=None, accum=None):
        if op1 is None:
            return S.op(eng, lambda e: e.tensor_scalar(out=out_, in0=in0, scalar1=s1, scalar2=None, op0=op0),
                        reads, writes)
        return S.op(eng, lambda e: e.tensor_scalar(out=out_, in0=in0, scalar1=s1, scalar2=s2, op0=op0, op1=op1,
                                                   accum_out=accum), reads, writes)

    def stt(out_, in0, scalar, in1, op0, op1, reads, writes):
        return S.op('dve', lambda e: e.scalar_tensor_tensor(out=out_, in0=in0, scalar=scalar, in1=in1,
                                                            op0=op0, op1=op1), reads, writes)

    def act(out_, in_, func, reads, writes, bias=None, scale=None, accum=None):
        def fn(e):
            kw = {}
            if bias is not None:
                kw['bias'] = bias
            if scale is not None:
                kw['scale'] = scale
            if accum is not None:
                kw['accum_out'] = accum
            return e.activation(out=out_, in_=in_, func=func, **kw)
        return S.op('act', fn, reads, writes)

    def cp(eng, out_, in_, reads, writes):
        if eng == 'act':
            return act(out_, in_, AF.Copy, reads, writes)
        return S.op(eng, lambda e: e.tensor_copy(out=out_, in_=in_), reads, writes)

    def recip(out_, in_, reads, writes):
        return S.op('dve', lambda e: e.reciprocal(out=out_, in_=in_), reads, writes)

    def memset(eng, ap, val, writes):
        return S.op(eng, lambda e: e.memset(ap, val), (), writes)

    def mmg(out_, pairs, reads, writes):
        def fn(e):
            ins = None
            n = len(pairs)
            for i, (l, r) in enumerate(pairs):
                ins = e.matmul(out_, l, r, start=(i == 0), stop=(i == n - 1))
            return ins
        return S.op('pe', fn, reads, writes)

    def trg(items, reads, writes):
        def fn(e):
            ins = None
            for (o, i_, idn) in items:
                ins = e.transpose(o, i_, idn)
            return ins
        return S.op('pe', fn, reads, writes)

    cfT = A.alloc([CFW], F32)
    dma('sp', cfT, cf, (), ['cf'])
    ident = cfT[:, 0:128]
    ones = cfT[:, 128:256]
    maskF = cfT[:, 256:384]
    maskB = cfT[:, 384:512]
    SU = cfT[:, 512:640]
    SL = cfT[:, 640:768]
    posw = cfT[:, 768:776]
    identb = A.alloc([128], BF16)
    cp('dve', identb, ident, ['cf'], ['identb'])
    ciT = A.alloc([NL], I32)
    dma('sp', ciT, ci, (), ['ci'])
    flagc = A.alloc([1], F32)
    dma('sp', flagc, flag[0].partition_broadcast(128), (), ['flag'])
    yfl = A.alloc([2], F32)
    dma('sp', yfl, yflag[0].partition_broadcast(128), (), ['yfl'])
    mark0 = A.top
    hT = A.alloc([8, TA], BF16)
    markH = A.top

    cv = A.alloc([16], F32)
    sv = A.alloc([16], F32)
    dma('sp', cv, cvec, (), ['cv'])
    act(sv, cv, AF.Silu, ['cv'], ['sv'])
    lhl = A.alloc([8, 128], F32)
    lhc = A.alloc([8, 128], F32)
    for k in range(8):
        ts('dve', lhl[:, k, :], ones, sv[:, k:k + 1], ALU.mult, ['sv', 'cf'], ['lhl'])
        ts('dve', lhc[:, k, :], ones, sv[:, 8 + k:9 + k], ALU.mult, ['sv', 'cf'], ['lhc'])
    modl = A.alloc([6 * D], F32)
    modc = A.alloc([2 * D], F32)
    wa = [A.alloc([8, 512], F32) for _ in range(2)]
    ba = [A.alloc([512], F32) for _ in range(2)]
    for nb in range(12):
        j = nb % 2
        dma('sp', wa[j], w_ada[:, nb * 512:(nb + 1) * 512].rearrange("(k p) n -> p k n", p=128), (), ['wa%d' % j])
        dma('sp', ba[j][0:1, :], b_ada[0:1, nb * 512:(nb + 1) * 512], (), ['ba%d' % j])
        prs = [(lhl[:, k, :], wa[j][:, k, :]) for k in range(8)] + [(ones[0:1, :], ba[j][0:1, :])]
        mmg(bank(j), prs, ['lhl', 'wa%d' % j, 'ba%d' % j, 'cf'], [BK(j)])
        cp('act', modl[:, nb * 512:(nb + 1) * 512], bank(j), [BK(j)], ['modl'])
        if nb < 4:
            prs = [(lhc[:, k, :], wa[j][:, k, :]) for k in range(8)] + [(ones[0:1, :], ba[j][0:1, :])]
            mmg(bank(2 + j), prs, ['lhc', 'wa%d' % j, 'ba%d' % j, 'cf'], [BK(2 + j)])
            cp('act', modc[:, nb * 512:(nb + 1) * 512], bank(2 + j), [BK(2 + j)], ['modc'])
    nwb = A.alloc([2, D], F32)
    dma('sp', nwb[:, 0, :], nw[0].partition_broadcast(128), (), ['nwb'])
    dma('sp', nwb[:, 1, :], nw[1].partition_broadcast(128), (), ['nwb'])
    stt(modl[:, D:2 * D], modl[:, D:2 * D], 1.0, nwb[:, 0, :], ALU.add, ALU.mult, ['modl', 'nwb'], ['modl'])
    stt(modc[:, D:2 * D], modc[:, D:2 * D], 1.0, nwb[:, 0, :], ALU.add, ALU.mult, ['modc', 'nwb'], ['modc'])
    stt(modl[:, 4 * D:5 * D], modl[:, 4 * D:5 * D], 1.0, nwb[:, 1, :], ALU.add, ALU.mult, ['modl', 'nwb'], ['modl'])
    dma('sp', mod_scr, modl, ['modl'], ['mod_scr'])

    xt = [A.alloc([D], F32) for _ in range(2)]
    t1 = [A.alloc([D], F32) for _ in range(2)]
    hb = [A.alloc([D], BF16) for _ in range(2)]
    junk = A.alloc([D], BF16)
    ssA = A.alloc([NCH], F32)
    sdA = A.alloc([NCH], F32)
    rsA = A.alloc([NCH], F32)
    for i in range(NCH):
        j = i % 2
        W1 = modc[:, D:2 * D] if i < 2 else modl[:, D:2 * D]
        B1 = modc[:, 0:D] if i < 2 else modl[:, 0:D]
        dma('sp', xt[j], xa[i * 128:(i + 1) * 128, :], (), ['xt%d' % j])
        act(junk, xt[j], AF.Square, ['xt%d' % j], ['junk', 'ssA%d' % i], accum=ssA[:, i:i + 1])
        act(sdA[:, i:i + 1], ssA[:, i:i + 1], AF.Sqrt, ['ssA%d' % i], ['sdA%d' % i], bias=EPS, scale=1.0 / D)
        recip(rsA[:, i:i + 1], sdA[:, i:i + 1], ['sdA%d' % i], ['rsA%d' % i])
        stt(t1[j], xt[j], rsA[:, i:i + 1], W1, ALU.mult, ALU.mult, ['xt%d' % j, 'rsA%d' % i, 'modl', 'modc'],
            ['t1%d' % j])
        tt('pool', hb[j], t1[j], B1, ALU.add, ['t1%d' % j, 'modl', 'modc'], ['hb%d' % j])
        b = 4 + j
        trg([(bankbf(b)[:, k * 128:(k + 1) * 128], hb[j][:, k * 128:(k + 1) * 128], identb) for k in range(8)],
            ['hb%d' % j, 'identb'], [BK(b)])
        cp('act', hT[:, :, i * 128:(i + 1) * 128], bankbf(b).rearrange("p (k t) -> p k t", k=8), [BK(b)], ['hT'])
    S.barrier()
    if stop_after == 'B':
        return _finish(nc, S, es, dbg, dbg_out, hT)
    A.top = markH

    wgb = A.alloc([8, 16], BF16)
    S.op('pool', lambda e: e.dma_start(out=wgb, in_=wg.rearrange("(k p) n -> p k n", p=128)), (), ['wgb'], dma=True)
    gb = A.alloc([16], F32)
    dma('sp', gb, gbias[0].partition_broadcast(128), (), ['gb'])
    lgb = A.alloc([8], F32)
    dma('sp', lgb, dlog[0].partition_broadcast(128), (), ['lgb'])
    hnwb = A.alloc([NU * 128], F32)
    dma('sp', hnwb, hnw[0].partition_broadcast(128), (), ['hnwb'])
    Gtok = A.alloc([NCH, 16], F32)
    Lt = A.alloc([NCH, 8], F32)
    tE = A.alloc([NCH, 8], F32)
    WC = A.alloc([NCH, 8], F32)
    LB = A.alloc([NCH, 8], F32)
    PHI = A.alloc([NCH, 8], F32)
    WCR = A.alloc([8], F32)
    RFR = A.alloc([8], F32)
    PHIR = A.alloc([8], F32)
    tR = A.alloc([8], F32)
    tR2 = A.alloc([8], F32)
    for c in range(NCH):
        if c < 32:
            o = bank(0)[:, c * 16:(c + 1) * 16]
            bk = BK(0)
        else:
            o = bank(1)[:, (c - 32) * 16:(c - 31) * 16]
            bk = BK(1)
        mmg(o, [(hT[:, k, c * 128:(c + 1) * 128], wgb[:, k, :]) for k in range(8)], ['hT', 'wgb'], [bk])
    gbb32 = gb[:, 0:16].unsqueeze(1).to_broadcast([128, 32, 16])
    gbb2 = gb[:, 0:16].unsqueeze(1).to_broadcast([128, 2, 16])
    tt('dve', Gtok[:, 0:32, :], bank(0).rearrange("p (c n) -> p c n", n=16), gbb32, ALU.add, [BK(0), 'gb'], ['Gtok'])
    tt('dve', Gtok[:, 32:34, :], bank(1)[:, 0:32].rearrange("p (c n) -> p c n", n=16), gbb2, ALU.add,
       [BK(1), 'gb'], ['Gtok'])
    act(tE, Gtok[:, :, 8:16], AF.Exp, ['Gtok'], ['tE'], scale=-1.0)
    act(Lt, tE, AF.Ln, ['tE'], ['Lt'], bias=1.0)
    for c in range(NCH):
        bk = 2 if c < 17 else 3
        o = bank(bk)[:, (c % 17) * 16:(c % 17) * 16 + 16]
        mmg(o[:, 0:4], [(SU, Lt[:, c, 0:4])], ['Lt', 'cf'], [BK(bk)])
        mmg(o[:, 4:8], [(SL, Lt[:, c, 4:8])], ['Lt', 'cf'], [BK(bk)])
        mmg(o[:, 8:16], [(ones, Lt[:, c, 0:8])], ['Lt', 'cf'], [BK(bk)])
    for (c0, c1, bk) in ((0, 17, 2), (17, 34, 3)):
        rp = bank(bk)[:, 0:17 * 16].rearrange("p (c n) -> p c n", n=16)
        tt('dve', tE[:, c0:c1, :], Gtok[:, c0:c1, 0:8], rp[:, :, 0:8], ALU.subtract, ['Gtok', BK(bk), 'Lt'], ['tE'])
        act(WC[:, c0:c1, :], tE[:, c0:c1, :], AF.Exp, ['tE'], ['WC'])
        act(LB[:, c0:c1, :], rp[:, :, 0:8], AF.Exp, [BK(bk)], ['LB'], scale=-1.0)
        act(PHI[:, c0:c1, :], rp[:, :, 8:16], AF.Exp, [BK(bk)], ['PHI'], scale=-1.0)
    act(tR, lgb, AF.Exp, ['lgb'], ['tR'], scale=-1.0)
    act(tR2, tR, AF.Ln, ['tR'], ['tR2'], bias=1.0)
    tt('dve', tR, posw, tR2, ALU.mult, ['tR2', 'cf', 'tR'], ['tR'])
    act(WCR, tR, AF.Exp, ['tR'], ['WCR'], scale=-1.0)
    act(RFR, tR, AF.Exp, ['tR'], ['RFR'])
    act(PHIR, tR2, AF.Exp, ['tR2'], ['PHIR'], scale=-128.0)

    wub = [A.alloc([8, 768], BF16) for _ in range(2)]
    QT = A.alloc([TA], BF16)
    KT = A.alloc([TA], BF16)
    Vext = A.alloc([NCH, 129], F32)
    Ktok = A.alloc([NCH, 128], BF16)
    OGs = A.alloc([NL, 128], BF16)
    Hs = A.alloc([NL, 128], F32)
    yv = [A.alloc([128], F32) for _ in range(2)]
    ya = [A.alloc([128], F32) for _ in range(2)]
    yb = [A.alloc([128], F32) for _ in range(2)]
    cosT = [A.alloc([512], F32) for _ in range(2)]
    sinT = [A.alloc([512], F32) for _ in range(2)]
    r1 = [A.alloc([512], F32) for _ in range(1)] * 2
    r2 = [A.alloc([512], F32) for _ in range(1)] * 2
    Ce = [A.alloc([129], F32) for _ in range(2)]
    Cd = [A.alloc([129], F32) for _ in range(2)]
    Cdb = [A.alloc([129], BF16) for _ in range(2)]
    Vw = [[A.alloc([129], BF16) for _ in range(2)] for _ in range(2)]
    Sm = [[A.alloc([128], BF16) for _ in range(2)] for _ in range(2)]
    denA = A.alloc([2, NCH], F32)
    recA = A.alloc([2, NCH], F32)
    ssq = A.alloc([NL], F32)
    sdq = A.alloc([NL], F32)
    rsq = A.alloc([NL], F32)
    ty = [A.alloc([128], F32) for _ in range(2)]
    memset('dve', Vext[:, :, 128:129], 1.0, ['Vext'])

    def load_wu(u):
        j = u % 2
        S.op('pool', lambda e: e.dma_start(out=wub[j], in_=winh[u].rearrange("(k p) n -> p k n", p=128)),
             (), ['wub%d' % j], dma=True)

    load_wu(0)
    order = [list(range(NCH)), [1, 0] + list(range(NCH - 1, 1, -1))]
    NBLK = (TA + 511) // 512
    for u in range(NU):
        mL = u < NU // 2
        wj = u % 2
        w = wub[wj]
        wk = 'wub%d' % wj
        if u + 1 < NU:
            load_wu(u + 1)
        for blk in range(NBLK):
            t0 = blk * 512
            n = min(512, TA - t0)
            j = blk % 2
            rhs = [hT[:, k, t0:t0 + n] for k in range(8)]
            if mL:
                bq, bk_ = j, 2 + j
                mmg(bank(bq)[:, 0:n], [(w[:, k, 0:128], rhs[k]) for k in range(8)], ['hT', wk], [BK(bq)])
                mmg(bank(bk_)[:, 0:n], [(w[:, k, 128:256], rhs[k]) for k in range(8)], ['hT', wk], [BK(bk_)])
                cp('act', QT[:, t0:t0 + n], bank(bq)[:, 0:n], [BK(bq)], ['QT'])
                act(KT[:, t0:t0 + n], bank(bk_)[:, 0:n], AF.Copy, [BK(bk_)], ['KT'], scale=KS)
            else:
                dma('sp', cosT[j][:, 0:n], rope[0][:, t0:t0 + n], (), ['cos%d' % j])
                dma('sp', sinT[j][:, 0:n], rope[1][:, t0:t0 + n], (), ['sin%d' % j])
                for (dst, dk, c0, sc) in ((QT, 'QT', 0, 1.0), (KT, 'KT', 128, KS)):
                    b0, b1 = (0, 1) if c0 == 0 else (2, 3)
                    mmg(bank(b0)[:, 0:n], [(w[:, k, c0:c0 + 128], rhs[k]) for k in range(8)], ['hT', wk], [BK(b0)])
                    mmg(bank(b1)[:, 0:n], [(w[:, k, 512 + c0:640 + c0], rhs[k]) for k in range(8)], ['hT', wk],
                        [BK(b1)])
                    stt(r1[j][:, 0:n], bank(b0)[:, 0:n], sc, cosT[j][:, 0:n], ALU.mult, ALU.mult,
                        [BK(b0), 'cos%d' % j], ['r1'])
                    stt(r2[j][:, 0:n], bank(b1)[:, 0:n], sc, sinT[j][:, 0:n], ALU.mult, ALU.mult,
                        [BK(b1), 'sin%d' % j], ['r2'])
                    tt('pool', dst[:, t0:t0 + n], r1[j][:, 0:n], r2[j][:, 0:n], ALU.add, ['r1', 'r2'],
                       [dk])
        for c in range(NCH):
            b = 4 + (c % 2)
            mmg(bank(b)[:, 0:256], [(hT[:, k, c * 128:(c + 1) * 128], w[:, k, 256:512]) for k in range(8)],
                ['hT', wk], [BK(b)])
            cp('act', Vext[:, c, 0:128], bank(b)[:, 0:128], [BK(b)], ['Vext'])
            if c >= 2:
                act(OGs[:, c - 2, :], bank(b)[:, 128:256], AF.Sigmoid if mL else AF.Silu, [BK(b)], ['OGs'])
        for c0 in range(0, NCH, 8):
            cs = list(range(c0, min(c0 + 8, NCH)))
            b = 6 + ((c0 // 8) % 2)
            trg([(bankbf(b)[:, i * 128:(i + 1) * 128], KT[:, c * 128:(c + 1) * 128], identb)
                 for i, c in enumerate(cs)], ['KT', 'identb'], [BK(b)])
            cp('act', Ktok[:, c0:c0 + len(cs), :],
               bankbf(b)[:, 0:len(cs) * 128].rearrange("p (c n) -> p c n", n=128), [BK(b)], ['Ktok'])
        for d in range(2):
            memset('dve', Ce[d], 0.0, ['Ce%d' % d])
        gk = ['PHI', 'WC', 'LB'] if mL else ['PHIR', 'WCR', 'RFR']

        def emit_out(d, c):
            L = c - 2
            jg = d * 4 + (u % 2)
            bO = 3 * d + 1
            first = (d == 0) == (L <= 15)
            hk = 'Hs%d' % L
            if mL:
                dn = denA[:, d, c:c + 1]
                rc = recA[:, d, c:c + 1]
                dk = 'den%d_%d' % (d, c)
                act(dn, bank(bO)[:, 128:129], AF.Abs, [BK(bO)], [dk])
                tt('dve', dn, dn, LB[:, c, jg:jg + 1], ALU.max, [dk] + gk, [dk])
                recip(rc, dn, [dk], ['r' + dk])
                scal = rc
                sck = ['r' + dk]
            else:
                scal = RFR[:, jg:jg + 1]
                sck = gk
            if first:
                ts('dve', Hs[:, L, :], bank(bO)[:, 0:128], scal, ALU.mult, [BK(bO)] + sck, [hk])
            else:
                stt(Hs[:, L, :], bank(bO)[:, 0:128], scal, Hs[:, L, :], ALU.mult, ALU.add,
                    [BK(bO), hk] + sck, [hk])

        pending = [None, None]
        for step in range(NCH):
            for d in range(2):
                c = order[d][step]
                jg = d * 4 + (u % 2)
                phi = PHI[:, c, jg:jg + 1] if mL else PHIR[:, jg:jg + 1]
                wc = WC[:, c, jg:jg + 1] if mL else WCR[:, jg:jg + 1]
                vj = step % 2
                vw = Vw[d][vj]
                vk = 'Vw%d%d' % (d, vj)
                bS, bO, bU = 3 * d, 3 * d + 1, 3 * d + 2
                tok = slice(c * 128, (c + 1) * 128)
                if c >= 2:
                    mmg(bank(bS)[:, 0:128], [(KT[:, tok], QT[:, tok])], ['KT', 'QT'], [BK(bS)])
                ts('dve', Cd[d], Ce[d], phi, ALU.mult, ['Ce%d' % d] + gk, ['Cd%d' % d])
                ts('dve', vw, Vext[:, c, :], wc, ALU.mult, ['Vext'] + gk, [vk])
                mmg(bank(bU)[:, 0:129], [(Ktok[:, c, :], vw)], ['Ktok', vk], [BK(bU)])
                if c >= 2:
                    cp('act', Cdb[d], Cd[d], ['Cd%d' % d], ['Cdb%d' % d])
                    sm = Sm[d][vj]
                    sk_ = 'Sm%d%d' % (d, vj)
                    tt('dve', sm, bank(bS)[:, 0:128], maskF if d == 0 else maskB, ALU.mult, [BK(bS), 'cf'], [sk_])
                tt('dve', Ce[d], Cd[d], bank(bU)[:, 0:129], ALU.add, ['Cd%d' % d, BK(bU)], ['Ce%d' % d])
                if pending[d] is not None:
                    emit_out(d, pending[d])
                    pending[d] = None
                if c >= 2:
                    mmg(bank(bO)[:, 0:129], [(sm, vw), (QT[:, tok], Cdb[d])], [sk_, vk, 'QT', 'Cdb%d' % d],
                        [BK(bO)])
                    pending[d] = c
        for d in range(2):
            if pending[d] is not None:
                emit_out(d, pending[d])
        for L in range(NL):
            act(junk[:, 0:128], Hs[:, L, :], AF.Square, ['Hs%d' % L], ['junk', 'ssq%d' % L], accum=ssq[:, L:L + 1])
        act(sdq, ssq, AF.Sqrt, ['ssq%d' % L for L in range(NL)], ['sdq'], bias=EPS, scale=1.0 / 128)
        recip(rsq, sdq, ['sdq'], ['rsq'])
        for L in range(NL):
            j = L % 2
            stt(ty[j], Hs[:, L, :], rsq[:, L:L + 1], hnwb[:, u * 128:(u + 1) * 128], ALU.mult, ALU.mult,
                ['Hs%d' % L, 'rsq', 'hnwb'], ['ty%d' % j])
            tt('dve', yv[j], ty[j], OGs[:, L, :], ALU.mult, ['ty%d' % j, 'OGs'], ['yv%d' % j])
            ts('dve', ya[j], yv[j], yfl[:, 0:1], ALU.mult, ['yv%d' % j, 'yfl'], ['ya%d' % j])
            ts('dve', yb[j], yv[j], yfl[:, 1:2], ALU.mult, ['yv%d' % j, 'yfl'], ['yb%d' % j])
            dma('sp', y_full[L * 128:(L + 1) * 128, u * 128:(u + 1) * 128], ya[j], ['ya%d' % j],
                ['yfa%d_%d' % (u, L)])
            dma('sp', y_full[L * 128:(L + 1) * 128, 512 + u * 128:512 + (u + 1) * 128], yb[j], ['yb%d' % j],
                ['yfb%d_%d' % (u, L)])
    S.barrier()
    for q in range(4):
        S.op('pool', lambda e, q=q: e.collective_compute(
            "AllReduce", ALU.add, replica_groups=RG, ins=[y_full[q * 1024:(q + 1) * 1024, :].opt()],
            outs=[y_ar[q * 1024:(q + 1) * 1024, :].opt()]), (), ['y_ar%d' % q], cc=True)
    S.barrier()
    if stop_after == 'C':
        return _finish(nc, S, es, dbg, dbg_out, None)

    A.top = (mark0 + 63) // 64 * 64
    H2X = A.alloc([NL, ROWW], BF16)
    AFF = A.alloc([NL, 16], F32)
    SLOT = A.alloc([NL, 16], I32)
    markD = A.top
    woutb = A.alloc([8, D], BF16)
    S.op('pool', lambda e: e.dma_start(out=woutb, in_=w_out.rearrange("(k p) n -> p k n", p=128)), (), ['woutb'],
         dma=True)
    wr = A.alloc([8, 16], F32)
    dma('sp', wr, w_router.rearrange("(k p) n -> p k n", p=128), (), ['wr'])
    G1 = A.alloc([D], F32)
    W2 = A.alloc([D], F32)
    B2 = A.alloc([D], F32)
    dma('sp', G1, mod_scr[:, 2 * D:3 * D], ['mod_scr'], ['G1'])
    dma('sp', W2, mod_scr[:, 4 * D:5 * D], ['mod_scr'], ['W2'])
    dma('sp', B2, mod_scr[:, 3 * D:4 * D], ['mod_scr'], ['B2'])
    ND = 3
    yt = [A.alloc([D], BF16) for _ in range(ND)]
    ytT = [A.alloc([8, 128], BF16) for _ in range(ND)]
    xt = [A.alloc([D], F32) for _ in range(ND)]
    tg = [A.alloc([D], F32) for _ in range(ND)]
    xl = [A.alloc([D], F32) for _ in range(ND)]
    xs = [A.alloc([D], F32) for _ in range(ND)]
    h2f = [A.alloc([D], F32) for _ in range(ND)]
    h2T = [A.alloc([8, 128], F32) for _ in range(ND)]
    junk = A.alloc([D], BF16)
    ss2 = A.alloc([NL], F32)
    sd2 = A.alloc([NL], F32)
    rs2 = A.alloc([NL], F32)
    mxA = A.alloc([NL], F32)
    seA = A.alloc([NL], F32)
    rseA = A.alloc([NL], F32)
    ex = [A.alloc([16], F32) for _ in range(ND)]
    for L in range(NL):
        j = L % ND
        jb = L % 2
        S.op('pool', lambda e, L=L, j=j: e.dma_start(out=yt[j], in_=y_ar[L * 128:(L + 1) * 128, :]), (),
             ['yt%d' % j], dma=True)
        dma('sp', xt[j], xa[TC + L * 128:TC + (L + 1) * 128, :], (), ['xt%d' % j])
        trg([(bankbf(jb)[:, k * 128:(k + 1) * 128], yt[j][:, k * 128:(k + 1) * 128], identb) for k in range(8)],
            ['yt%d' % j, 'identb'], [BK(jb)])
        cp('act', ytT[j], bankbf(jb).rearrange("p (k t) -> p k t", k=8), [BK(jb)], ['ytT%d' % j])
        for h in range(2):
            b = 2 + h
            mmg(bank(b), [(ytT[j][:, k, :], woutb[:, k, h * 512:(h + 1) * 512]) for k in range(8)],
                ['ytT%d' % j, 'woutb'], [BK(b)])
            tt('dve', tg[j][:, h * 512:(h + 1) * 512], bank(b), G1[:, h * 512:(h + 1) * 512], ALU.mult,
               [BK(b), 'G1'], ['tg%d' % j])
        tt('dve', xl[j], tg[j], xt[j], ALU.add, ['tg%d' % j, 'xt%d' % j], ['xl%d' % j])
        act(xs[j], xl[j], AF.Copy, ['xl%d' % j], ['xs%d' % j], scale=0.5 if use_ar else 1.0)
        dma('sp', xl_scr[L * 128:(L + 1) * 128, :], xs[j], ['xs%d' % j], ['xls%d' % L])
        act(junk, xl[j], AF.Square, ['xl%d' % j], ['junk', 'ss2%d' % L], accum=ss2[:, L:L + 1])
        act(sd2[:, L:L + 1], ss2[:, L:L + 1], AF.Sqrt, ['ss2%d' % L], ['sd2%d' % L], bias=EPS, scale=1.0 / D)
        recip(rs2[:, L:L + 1], sd2[:, L:L + 1], ['sd2%d' % L], ['rs2%d' % L])
        stt(tg[j], xl[j], rs2[:, L:L + 1], W2, ALU.mult, ALU.mult, ['xl%d' % j, 'rs2%d' % L, 'W2', 'tg%d' % j],
            ['tg%d' % j])
        tt('dve', h2f[j], tg[j], B2, ALU.add, ['tg%d' % j, 'B2'], ['h2f%d' % j])
        cp('act', H2X[:, L, 0:D], h2f[j], ['h2f%d' % j], ['H2Xa%d' % L])
        for h in range(2):
            b = 4 + h
            trg([(bank(b)[:, i * 128:(i + 1) * 128], h2f[j][:, (4 * h + i) * 128:(4 * h + i + 1) * 128], ident)
                 for i in range(4)], ['h2f%d' % j, 'cf'], [BK(b)])
            cp('act', h2T[j][:, 4 * h:4 * h + 4, :], bank(b).rearrange("p (k t) -> p k t", k=4), [BK(b)],
               ['h2T%d' % j])
        b = 6 + jb
        mmg(bank(b)[:, 0:16], [(h2T[j][:, k, :], wr[:, k, :]) for k in range(8)], ['h2T%d' % j, 'wr'], [BK(b)])
        S.op('dve', lambda e, o=mxA[:, L:L + 1], i_=bank(b)[:, 0:16]: e.tensor_reduce(out=o, in_=i_, axis=AX.X,
                                                                                     op=ALU.max, negate=True),
             [BK(b)], ['mx%d' % L])
        act(ex[j], bank(b)[:, 0:16], AF.Exp, [BK(b), 'mx%d' % L], ['ex%d' % j, 'se%d' % L], bias=mxA[:, L:L + 1],
            accum=seA[:, L:L + 1])
        recip(rseA[:, L:L + 1], seA[:, L:L + 1], ['se%d' % L], ['rse%d' % L])
        ts('dve', AFF[:, L, :], ex[j], rseA[:, L:L + 1], ALU.mult, ['ex%d' % j, 'rse%d' % L], ['AFF%d' % L])
        cp('dve', H2X[:, L, D:D + 32].bitcast(F32), AFF[:, L, :], ['AFF%d' % L], ['H2Xb%d' % L])
        cp('dve', H2X[:, L, D + 32:D + 34].bitcast(I32), ciT[:, L:L + 1], ['ci'], ['H2Xc%d' % L])
    S.barrier()
    if stop_after == 'D':
        return _finish(nc, S, es, dbg, dbg_out, None)

    A.top = markD
    affT = A.alloc([T], F32)
    msk = A.alloc([T], F32)
    pos = A.alloc([T], F32)
    one16 = A.alloc([T], F32)
    sc16 = A.alloc([16], F32)
    lo, hi, sm_, mid, cnt, ge, u1, u2 = [sc16[0:16, i:i + 1] for i in range(8)]
    for g in range(8):
        b = g % 2
        trg([(bank(b)[0:16, i * 128:(i + 1) * 128], AFF[:, 4 * g + i, :], ident) for i in range(4)], ['AFF', 'cf'],
            [BK(b)])
        cp('act', affT[0:16, g * 512:(g + 1) * 512], bank(b)[0:16, :], [BK(b)], ['affT'])
    memset('dve', sc16[0:16, :], 0.0, ['sc16'])
    memset('dve', hi, 1.0, ['sc16'])
    memset('dve', one16[0:16, :], 1.0, ['one16'])
    for it in range(NIT):
        tt('dve', sm_, lo, hi, ALU.add, ['sc16'], ['sc16'])
        ts('dve', mid, sm_, 0.5, ALU.mult, ['sc16'], ['sc16'])
        ts('dve', msk[0:16, :], affT[0:16, :], mid, ALU.is_ge, ['affT', 'sc16'], ['msk', 'sc16'], s2=None,
           op1=ALU.add, accum=cnt)
        ts('dve', ge, cnt, CAP - 0.5, ALU.is_ge, ['sc16'], ['sc16'])
        tt('dve', u1, ge, mid, ALU.mult, ['sc16'], ['sc16'])
        tt('dve', lo, lo, u1, ALU.max, ['sc16'], ['sc16'])
        stt(u2, ge, 4.0, mid, ALU.mult, ALU.add, ['sc16'], ['sc16'])
        tt('dve', hi, hi, u2, ALU.min, ['sc16'], ['sc16'])
    ts('dve', msk[0:16, :], affT[0:16, :], lo, ALU.is_ge, ['affT', 'sc16'], ['msk'])
    S.op('dve', lambda e: e.tensor_tensor_scan(out=pos[0:16, :], data0=one16[0:16, :], data1=msk[0:16, :],
                                               initial=0.0, op0=ALU.mult, op1=ALU.add), ['msk', 'one16'], ['pos'])
    stt(pos[0:16, :], pos[0:16, :], -1001.0, msk[0:16, :], ALU.add, ALU.mult, ['pos', 'msk'], ['pos'])
    ts('dve', pos[0:16, :], pos[0:16, :], 1000.0, ALU.add, ['pos'], ['pos'])
    trg([(bank(2)[:, L * 16:(L + 1) * 16], pos[0:16, L * 128:(L + 1) * 128], ident[0:16, 0:16]) for L in range(NL)],
        ['pos', 'cf'], [BK(2)])
    cp('dve', SLOT, bank(2).rearrange("p (c n) -> p c n", n=16), [BK(2)], ['SLOT'])
    S.barrier()
    if stop_after == 'E':
        return _finish(nc, S, es, dbg, dbg_out, None)

    A.top = markD
    G2 = A.alloc([D], F32)
    dma('sp', G2, mod_scr[:, 5 * D:6 * D], ['mod_scr'], ['G2'])
    Wd = A.alloc([NFC, D], BF16)
    NWB = 3
    Wgs = [A.alloc([8, 256], BF16) for _ in range(NWB)]
    Wus = [A.alloc([8, 256], BF16) for _ in range(NWB)]
    actb = A.alloc([NFC, CAP], BF16)
    XG = [A.alloc([4, ROWW], BF16) for _ in range(2)]
    xgT = [A.alloc([8, CAP], BF16) for _ in range(1)]
    yo = [A.alloc([D], F32) for _ in range(2)]
    sa = [A.alloc([CAP], BF16) for _ in range(2)]
    regs = {}

    def bcreg(e):
        if 'bc' not in regs:
            regs['bc'] = e.alloc_register('bc')
            e.reg_mov(regs['bc'], CAP - 1)
        return regs['bc']

    prev_add = []
    wcount = 0
    ycount = 0

    def scatter_rows(e_, Ls):
        for L in Ls:
            S.op('pool', lambda e, L=L, e_=e_: e.indirect_dma_start(
                out=xg_flat, out_offset=bass.IndirectOffsetOnAxis(ap=SLOT[:, L, e_:e_ + 1], axis=0),
                in_=H2X[:, L, :], in_offset=None, element_offset=e_ * CAP * ROWW, bounds_check=bcreg(e),
                oob_is_err=False),
                ['H2X', 'SLOT'], ['xgs%d_%d' % (e_, L)], dma=True)

    def load_wd(e_):
        for hh in range(2):
            S.op('pool', lambda e, hh=hh, e_=e_: e.dma_start(
                out=Wd[:, hh * 11:(hh + 1) * 11, :],
                in_=w_down[e_][hh * 1408:(hh + 1) * 1408, :].rearrange("(f p) n -> p f n", p=128)),
                (), ['Wd'], dma=True)

    scatter_rows(0, range(NL))
    for e_ in range(NEL):
        xj = e_ % 2
        dma('sp', XG[xj], xg_scr[e_].rearrange("(s p) n -> p s n", p=128), ['xgs%d_%d' % (e_, L) for L in range(NL)],
            ['XG%d' % xj])
        for k0 in range(0, 8, 2):
            b = 6 + ((k0 // 2) % 2)
            trg([(bankbf(b)[:, (kk * 4 + s_) * 128:(kk * 4 + s_ + 1) * 128],
                  XG[xj][:, s_, (k0 + kk) * 128:(k0 + kk + 1) * 128], identb) for kk in range(2) for s_ in range(4)],
                ['XG%d' % xj, 'identb'], [BK(b)])
            cp('act', xgT[0][:, k0:k0 + 2, :], bankbf(b).rearrange("p (k t) -> p k t", k=2), [BK(b)],
               ['xgT0'])
        for fb in range(NFC // 2):
            wj = wcount % NWB
            wcount += 1
            S.op('pool', lambda e, fb=fb, e_=e_, wj=wj: e.dma_start(
                out=Wgs[wj], in_=w_gate[e_][:, fb * 256:(fb + 1) * 256].rearrange("(k p) n -> p k n", p=128)),
                (), ['Wgs%d' % wj], dma=True)
            S.op('pool', lambda e, fb=fb, e_=e_, wj=wj: e.dma_start(
                out=Wus[wj], in_=w_up[e_][:, fb * 256:(fb + 1) * 256].rearrange("(k p) n -> p k n", p=128)),
                (), ['Wus%d' % wj], dma=True)
            if fb == 2:
                load_wd(e_)
            if e_ + 1 < NEL:
                scatter_rows(e_ + 1, range(fb * 3, min(NL, fb * 3 + 3)))
            for fc in range(2):
                fi = fb * 2 + fc
                j = fi % 2
                mmg(bank(j), [(Wgs[wj][:, k, fc * 128:(fc + 1) * 128], xgT[0][:, k, :]) for k in range(8)],
                    ['Wgs%d' % wj, 'xgT0'], [BK(j)])
                mmg(bank(2 + j), [(Wus[wj][:, k, fc * 128:(fc + 1) * 128], xgT[0][:, k, :]) for k in range(8)],
                    ['Wus%d' % wj, 'xgT0'], [BK(2 + j)])
                act(sa[j], bank(j), AF.Silu, [BK(j)], ['sa%d' % j])
                tt('dve', actb[:, fi, :], sa[j], bank(2 + j), ALU.mult, ['sa%d' % j, BK(2 + j)], ['actb'])
        cur_add = []
        for s_ in range(4):
            yj = ycount % 2
            ycount += 1
            gcol = XG[xj][:, s_, D:D + 32].bitcast(F32)[:, e_:e_ + 1]
            icol = XG[xj][:, s_, D + 32:D + 34].bitcast(I32)
            for h in range(2):
                b = 4 + h
                mmg(bank(b), [(actb[:, f, s_ * 128:(s_ + 1) * 128], Wd[:, f, h * 512:(h + 1) * 512]) for f in range(NFC)],
                    ['actb', 'Wd'], [BK(b)])
                stt(yo[yj][:, h * 512:(h + 1) * 512], bank(b), gcol, G2[:, h * 512:(h + 1) * 512], ALU.mult, ALU.mult,
                    [BK(b), 'XG%d' % xj, 'G2'], ['yo%d' % yj])
            m = S.op('pool', lambda e, yj=yj, icol=icol: e.indirect_dma_start(
                out=xl_scr, out_offset=bass.IndirectOffsetOnAxis(ap=icol, axis=0), in_=yo[yj], in_offset=None,
                compute_op=ALU.add), ['yo%d' % yj, 'XG%d' % xj, 'xl_scr'], ['add%d_%d' % (e_, s_)], dma=True,
                extra=prev_add)
            cur_add.append(m)
        prev_add = cur_add
    S.barrier()
    if use_ar:
        for q in range(4):
            S.op('pool', lambda e, q=q: e.collective_compute(
                "AllReduce", ALU.add, replica_groups=RG, ins=[xl_scr[q * 1024:(q + 1) * 1024, :].opt()],
                outs=[xl_ar[q * 1024:(q + 1) * 1024, :].opt()]), (), ['xl_ar%d' % q], cc=True)

    A.top = markD
    nw3 = A.alloc([D], F32)
    dma('sp', nw3, nw[2].partition_broadcast(128), (), ['nw3'])
    xf = [A.alloc([D], F32) for _ in range(2)]
    of = [A.alloc([D], F32) for _ in range(2)]
    junk = A.alloc([D], BF16)
    ss3 = A.alloc([NL], F32)
    sd3 = A.alloc([NL], F32)
    rs3 = A.alloc([NL], F32)
    for L in range(NL):
        j = L % 2
        dma('sp', xf[j], xl_ar[L * 128:(L + 1) * 128, :], ['xl_ar%d' % (L // 8)], ['xf%d' % j])
        act(junk, xf[j], AF.Square, ['xf%d' % j], ['junk', 'ss3%d' % L], accum=ss3[:, L:L + 1])
        act(sd3[:, L:L + 1], ss3[:, L:L + 1], AF.Sqrt, ['ss3%d' % L], ['sd3%d' % L], bias=EPS, scale=1.0 / D)
        recip(rs3[:, L:L + 1], sd3[:, L:L + 1], ['sd3%d' % L], ['rs3%d' % L])
        stt(of[j], xf[j], rs3[:, L:L + 1], nw3, ALU.mult, ALU.mult, ['xf%d' % j, 'rs3%d' % L, 'nw3'], ['of%d' % j])
        dma('sp', out[L * 128:(L + 1) * 128, :], of[j], ['of%d' % j], ['out%d' % L])
    return _finish(nc, S, es, dbg, dbg_out, None)


def _end_of(ap, arena_t):
    return int(ap.offset - arena_t[:, 0:1].offset) + int(np.prod(ap.shape[1:])) * DSZ[ap.dtype]


def _finish(nc, S, es, dbg, dbg_out, _unused):
    if dbg is not None:
        S.op('sp', lambda e: e.dma_start(out=dbg_out, in_=dbg[0]()), (), ['dbg'], dma=True)
    S.barrier()
    S.emit()
    es.close()
    return nc


def _consts():
    p = np.arange(128)
    cf = np.zeros((128, CFW), np.float32)
    cf[:, 0:128] = np.eye(128)
    cf[:, 128:256] = 1.0
    cf[:, 256:384] = (p[:, None] <= p[None, :])
    cf[:, 384:512] = (p[:, None] >= p[None, :])
    cf[:, 512:640] = (p[:, None] > p[None, :])
    cf[:, 640:768] = (p[:, None] < p[None, :])
    cf[:, 768:772] = (127 - p)[:, None]
    cf[:, 772:776] = p[:, None]
    ci = (np.arange(NL)[None, :] * 128 + p[:, None]).astype(np.int32)
    t = np.arange(T)
    rows = (t // 64).astype(np.float32)
    cols = (t % 64).astype(np.float32)
    inv = (np.float32(10000.0) ** (-np.arange(32, dtype=np.float32) / np.float32(32))).astype(np.float32)
    ar = (rows[:, None] * inv[None, :]).astype(np.float32)
    ac = (cols[:, None] * inv[None, :]).astype(np.float32)
    C = np.ones((128, TA), np.float32)
    Sn = np.zeros((128, TA), np.float32)
    for i in range(128):
        ang = ar[:, i % 32] if i < 64 else ac[:, i % 32]
        C[i, TC:] = np.cos(ang)
        Sn[i, TC:] = np.sin(ang) * (-1.0 if (i % 64) < 32 else 1.0)
    rope = np.stack([C, Sn]).astype(np.float32)
    perm = np.array([i + 32 if (i % 64) < 32 else i - 32 for i in range(128)])
    return cf, ci, rope, perm


def _prep(inputs, NEL, ncores):
    f = lambda a: np.ascontiguousarray(np.asarray(a, dtype=np.float32))
    x, c, ctx, c_ctx = f(inputs['x']), f(inputs['c']), f(inputs['ctx']), f(inputs['c_ctx'])
    w_in = f(inputs['w_in'])[0]
    cf, ci, rope, perm = _consts()
    winh = np.zeros((8, D, 768), np.float32)
    for u in range(8):
        if u < 4:
            base = [0, 512, 1024, 1536]
            h = u
        else:
            base = [2064, 2576, 3088, 3600]
            h = u - 4
        for i, b in enumerate(base):
            winh[u, :, i * 128:(i + 1) * 128] = w_in[:, b + h * 128:b + (h + 1) * 128]
        if u >= 4:
            winh[u, :, 512:640] = winh[u, :, 0:128][:, perm]
            winh[u, :, 640:768] = winh[u, :, 128:256][:, perm]
    wg_all = np.ascontiguousarray(w_in[:, 2048:2064])
    gb_all = f(inputs['mlstm_gate_bias'])[0]
    dl_all = f(inputs['ret_decay_logit'])[0].reshape(8)
    mnw, rnw = f(inputs['mlstm_norm_w'])[0], f(inputs['ret_norm_w'])[0]
    w_out_full = f(inputs['w_out'])[0]
    rows = []
    for gg in range(2):
        for h in (2 * gg, 2 * gg + 1):
            rows += list(range(h * 128, (h + 1) * 128))
        for h in (2 * gg, 2 * gg + 1):
            rows += list(range(512 + h * 128, 512 + (h + 1) * 128))
    shared = dict(
        w_ada=f(inputs['w_ada'])[0], b_ada=f(inputs['b_ada'])[0][None],
        nw=np.stack([f(inputs['norm1_w'])[0], f(inputs['norm2_w'])[0], f(inputs['final_norm_w'])]),
        w_out=np.ascontiguousarray(w_out_full[rows, :]), rope=rope, cf=cf, ci=ci)
    percore = []
    for g in range(2):
        hl = [2 * g, 2 * g + 1]
        hp = hl + [h for h in range(4) if h not in hl]
        gcols = [t * 4 + h for t in range(4) for h in hp]
        dcols = [dr * 4 + h for dr in range(2) for h in hp]
        percore.append(dict(
            winh=np.ascontiguousarray(winh[[hl[0], hl[1], 4 + hl[0], 4 + hl[1]]]),
            wg=np.ascontiguousarray(wg_all[:, gcols]), gbias=np.ascontiguousarray(gb_all[gcols])[None],
            dlog=np.ascontiguousarray(dl_all[dcols])[None],
            hnw=np.concatenate([mnw[hl[0] * 128:(hl[1] + 1) * 128], rnw[hl[0] * 128:(hl[1] + 1) * 128]])[None],
            yflag=np.array([[1.0, 0.0]] if g == 0 else [[0.0, 1.0]], np.float32)))
    wr = f(inputs['w_router'])[0]
    wgate, wup, wdown = f(inputs['w_gate'])[0], f(inputs['w_up'])[0], f(inputs['w_down'])[0]
    maps = []
    ngrp = NE // NEL
    for core in range(ncores):
        b = core // ngrp
        g = core % ngrp
        eidx = list(range(g * NEL, (g + 1) * NEL)) + [e for e in range(NE) if not (g * NEL <= e < (g + 1) * NEL)]
        m = dict(shared)
        m.update(percore[g])
        m['xa'] = np.concatenate([ctx[b], x[b]], axis=0)
        cv = np.zeros((128, 16), np.float32)
        cv[:, 0:8] = c[b].reshape(8, 128).T
        cv[:, 8:16] = c_ctx.reshape(8, 128).T
        m['cvec'] = cv
        m['w_router'] = np.ascontiguousarray(wr[:, eidx])
        m['w_gate'] = np.ascontiguousarray(wgate[g * NEL:(g + 1) * NEL])
        m['w_up'] = np.ascontiguousarray(wup[g * NEL:(g + 1) * NEL])
        m['w_down'] = np.ascontiguousarray(wdown[g * NEL:(g + 1) * NEL])
        m['flag'] = np.array([[1.0 if g == 0 else 0.0]], np.float32)
        maps.append(m)
    return maps


NEL_DEFAULT = 8
NCORES = 8


def kernel(**inputs):
    ngrp = NE // NEL_DEFAULT
    nc = build_nc(NEL_DEFAULT, use_ar=(ngrp > 1))
    maps = _prep(inputs, NEL_DEFAULT, NCORES)
    res = run_bass_kernel_spmd(nc, maps, core_ids=list(range(NCORES)))
    outs = [np.asarray(res.results[b * ngrp]["out"], dtype=np.float32) for b in range(4)]
    return np.stack(outs, axis=0)
```
